# Optimizing a Trainium2 kernel written in Bass

```python
import math
import jax, jax.numpy as jnp
from jax import lax
import numpy as np

D_MODEL = 1024
BATCH = 2
SEQ = 8192
DEPTH = 2

GRID_W = 64
CTX_LEN = 256
EPS = 1e-6

DA_HEADS = 4
DA_QK = 64
DA_V = 2 * DA_QK
DA_WIDTH = DA_HEADS * DA_V
ROPE_THETA = 10000.0
Q_BLOCK = 128

SSD_HEADS = 4
SSD_HEAD_DIM = 64
SSD_WIDTH = SSD_HEADS * SSD_HEAD_DIM
SSD_GROUPS = 2
SSD_STATE = 64
SSD_CONV = 3
SSD_CONV_CH = SSD_WIDTH + 2 * SSD_GROUPS * SSD_STATE
SSD_CHUNK = 128

GLA_HEADS = 4
GLA_DK = 32
GLA_DV = 64
GLA_KEY_WIDTH = GLA_HEADS * GLA_DK
GLA_WIDTH = GLA_HEADS * GLA_DV
GLA_RANK = 16
GLA_NORMALIZER = 16.0
GLA_CHUNK = 64

MIX_WIDTH = DA_WIDTH + SSD_WIDTH + GLA_WIDTH

IN_SIZES = (
    DA_HEADS * 2 * DA_QK,
    DA_HEADS * 2 * DA_QK,
    DA_WIDTH,
    SSD_WIDTH,
    SSD_CONV_CH,
    2 * SSD_HEADS,
    GLA_KEY_WIDTH,
    GLA_KEY_WIDTH,
    GLA_WIDTH,
    GLA_WIDTH,
    2 * GLA_RANK,
)
IN_WIDTH = 3112

N_EXPERTS = 32
TOP_K = 4
D_EXPERT = 1024
SWIGLU_LIMIT = 7.0
SWIGLU_ALPHA = 1.702
MOE_BLOCK = 128

kernel_name = "hybrid_diffattn_ssd_gla_moe_block"


def _rms(xf):
    return xf * lax.rsqrt(jnp.mean(xf * xf, axis=-1, keepdims=True) + EPS)


def rms_norm(x, w):
    return (_rms(x.astype(jnp.float32)) * w.astype(jnp.float32)).astype(x.dtype)


def split_cols(p):
    outs = []
    start = 0
    for n in IN_SIZES:
        outs.append(p[..., start:start + n])
        start += n
    return outs


def rope_1d(x, pos):
    f = x.shape[-1] // 2
    inv = ROPE_THETA ** (-jnp.arange(f, dtype=jnp.float32) / f)
    ang = pos.astype(jnp.float32)[:, None] * inv
    cos = jnp.cos(ang)[:, None, None, :]
    sin = jnp.sin(ang)[:, None, None, :]
    xf = x.astype(jnp.float32)
    x1, x2 = xf[..., :f], xf[..., f:]
    return jnp.concatenate([x1 * cos - x2 * sin, x2 * cos + x1 * sin], axis=-1).astype(x.dtype)


def axial_rope(x, row, col):
    half = x.shape[-1] // 2
    return jnp.concatenate([rope_1d(x[..., :half], row), rope_1d(x[..., half:], col)], axis=-1)


def diff_softmax_attend(q, k, v, lam):
    s = jnp.einsum('bqhmd,bkhmd->bhmqk', q, k, preferred_element_type=jnp.float32) * (DA_QK ** -0.5)
    p = jax.nn.softmax(s, axis=-1)
    pd = p[:, :, 0] - lam * p[:, :, 1]
    return jnp.einsum('bhqk,bkhe->bqhe', pd.astype(v.dtype), v)


def diff_attention_latent(q, k_all, v_all, lam):
    Bsz, S = q.shape[0], q.shape[1]
    nb = S // Q_BLOCK
    qb = jnp.moveaxis(q.reshape(Bsz, nb, Q_BLOCK, DA_HEADS, 2, DA_QK), 1, 0)
    ob = lax.map(lambda qi: diff_softmax_attend(qi, k_all, v_all, lam), qb)
    return jnp.moveaxis(ob, 0, 1).reshape(Bsz, S, DA_HEADS, DA_V)


def centred_dwconv(x, w, b):
    K = w.shape[0]
    y = lax.conv_general_dilated(x, w[:, None, :].astype(x.dtype), window_strides=(1,),
                                 padding=[(K // 2, K // 2)],
                                 dimension_numbers=('NWC', 'WIO', 'NWC'),
                                 feature_group_count=x.shape[-1])
    return y + b.astype(x.dtype)


def ssd_chunked(x, dt, A, Bm, Cm, h0):
    Bsz, L, H, P = x.shape
    G, N = Bm.shape[2], Bm.shape[3]
    Q = SSD_CHUNK
    nc = L // Q
    rep = H // G
    Bh = jnp.repeat(Bm, rep, axis=2).reshape(Bsz, nc, Q, H, N)
    Ch = jnp.repeat(Cm, rep, axis=2).reshape(Bsz, nc, Q, H, N)
    xc = x.reshape(Bsz, nc, Q, H, P)
    dtc = dt.reshape(Bsz, nc, Q, H)
    acs = jnp.cumsum(dtc * A, axis=2)
    seg = acs[:, :, :, None, :] - acs[:, :, None, :, :]
    lower = jnp.tril(jnp.ones((Q, Q), dtype=bool))[None, None, :, :, None]
    decay = jnp.exp(jnp.where(lower, seg, -jnp.inf))
    scores = jnp.einsum('bcihn,bcjhn->bcijh', Ch, Bh) * decay * dtc[:, :, None, :, :]
    y = jnp.einsum('bcijh,bcjhp->bcihp', scores, xc)
    w_end = jnp.exp(acs[:, :, -1:, :] - acs) * dtc
    states = jnp.einsum('bcjh,bcjhn,bcjhp->bchpn', w_end, Bh, xc)
    chunk_decay = jnp.exp(acs[:, :, -1, :])

    def step(h, inp):
        s, d = inp
        return h * d[:, :, None, None] + s, h

    hT, h_in = lax.scan(step, h0, (jnp.moveaxis(states, 1, 0), jnp.moveaxis(chunk_decay, 1, 0)))
    h_in = jnp.moveaxis(h_in, 0, 1)
    y = y + jnp.einsum('bcihn,bcih,bchpn->bcihp', Ch, jnp.exp(acs), h_in)
    return y.reshape(Bsz, L, H, P), hT


def ssd_sequence(xbc, dt_raw, conv_w, conv_b, a_log, dt_bias, d_skip, h0):
    Bsz, L = xbc.shape[0], xbc.shape[1]
    GN = SSD_GROUPS * SSD_STATE
    xbc = jax.nn.silu(centred_dwconv(xbc, conv_w, conv_b)).astype(jnp.float32)
    xs = xbc[..., :SSD_WIDTH].reshape(Bsz, L, SSD_HEADS, SSD_HEAD_DIM)
    Bm = xbc[..., SSD_WIDTH:SSD_WIDTH + GN].reshape(Bsz, L, SSD_GROUPS, SSD_STATE)
    Cm = xbc[..., SSD_WIDTH + GN:].reshape(Bsz, L, SSD_GROUPS, SSD_STATE)
    dt = jax.nn.softplus(dt_raw.astype(jnp.float32).reshape(Bsz, L, 2, SSD_HEADS) + dt_bias.astype(jnp.float32))
    A = -jnp.exp(a_log.astype(jnp.float32))
    fl = lambda t: jnp.flip(t, axis=1)
    y_f, h_f = ssd_chunked(xs, dt[:, :, 0], A[0], Bm, Cm, h0[0])
    y_b, h_b = ssd_chunked(fl(xs), fl(dt[:, :, 1]), A[1], fl(Bm), fl(Cm), h0[1])
    y = y_f + fl(y_b) + d_skip.astype(jnp.float32)[:, None] * xs
    return y.reshape(Bsz, L, SSD_WIDTH), (h_f, h_b)


def ssd_gated_norm(y, z, w):
    g = y * jax.nn.silu(z.astype(jnp.float32))
    gs = _rms(g.reshape(*g.shape[:-1], SSD_GROUPS, SSD_WIDTH // SSD_GROUPS)).reshape(g.shape)
    return (gs * w.astype(jnp.float32)).astype(z.dtype)


def gla_chunked(q, k, v, gk, h0):
    Bsz, L, H, dk = q.shape
    dv = v.shape[-1]
    Q = GLA_CHUNK
    nc = L // Q
    qc = q.reshape(Bsz, nc, Q, H, dk)
    kc = k.reshape(Bsz, nc, Q, H, dk)
    vc = v.reshape(Bsz, nc, Q, H, dv)
    b = jnp.cumsum(gk.reshape(Bsz, nc, Q, H, dk), axis=2)
    q_t = qc * jnp.exp(b)
    k_t = kc * jnp.exp(-b)
    lower = jnp.tril(jnp.ones((Q, Q), dtype=bool))
    att = jnp.where(lower, jnp.einsum('bcihd,bcjhd->bchij', q_t, k_t), 0.0)
    o = jnp.einsum('bchij,bcjhv->bcihv', att, vc)
    k_end = kc * jnp.exp(b[:, :, -1:] - b)
    states = jnp.einsum('bcjhd,bcjhv->bchdv', k_end, vc)
    chunk_decay = jnp.exp(b[:, :, -1])

    def step(h, inp):
        s, d = inp
        return h * d[..., None] + s, h

    hT, h_in = lax.scan(step, h0, (jnp.moveaxis(states, 1, 0), jnp.moveaxis(chunk_decay, 1, 0)))
    h_in = jnp.moveaxis(h_in, 0, 1)
    o = o + jnp.einsum('bcihd,bchdv->bcihv', q_t, h_in)
    return o.reshape(Bsz, L, H, dv), hT


def gla_sequence(q, k, v, code, gk_up, gk_b, h0):
    Bsz, L = q.shape[0], q.shape[1]
    f32 = jnp.float32
    q = q.astype(f32).reshape(Bsz, L, GLA_HEADS, GLA_DK) * (GLA_DK ** -0.5)
    k = k.astype(f32).reshape(Bsz, L, GLA_HEADS, GLA_DK)
    v = v.astype(f32).reshape(Bsz, L, GLA_HEADS, GLA_DV)
    code = code.astype(f32).reshape(Bsz, L, 2, GLA_RANK)
    gk = jax.nn.log_sigmoid(jnp.einsum('bldr,drk->bldk', code, gk_up.astype(f32)) + gk_b.astype(f32)) / GLA_NORMALIZER
    gk = gk.reshape(Bsz, L, 2, GLA_HEADS, GLA_DK)
    fl = lambda t: jnp.flip(t, axis=1)
    o_f, s_f = gla_chunked(q, k, v, gk[:, :, 0], h0[0])
    o_b, s_b = gla_chunked(fl(q), fl(k), fl(v), fl(gk[:, :, 1]), h0[1])
    return o_f + fl(o_b), (s_f, s_b)


def gla_finish(o, g, w):
    Bsz, L = o.shape[0], o.shape[1]
    on = _rms(o) * w.astype(jnp.float32)
    gg = jax.nn.silu(g.astype(jnp.float32).reshape(Bsz, L, GLA_HEADS, GLA_DV))
    return (on * gg).reshape(Bsz, L, GLA_WIDTH).astype(g.dtype)


def moe_ffn(h, w_router, b_router, w_gate_up, b_gate_up, w_down, b_down):
    T, D = h.shape
    logits = jnp.dot(h, w_router).astype(jnp.float32) + b_router.astype(jnp.float32)
    top_v, top_i = lax.top_k(logits, TOP_K)
    gates = jax.nn.softmax(top_v, axis=-1)
    TK = T * TOP_K
    flat_e = top_i.reshape(-1).astype(jnp.int32)
    flat_tok = jnp.arange(TK, dtype=jnp.int32) // TOP_K
    order = jnp.argsort(flat_e)
    sorted_e = flat_e[order]
    sorted_tok = flat_tok[order]
    sorted_gate = gates.reshape(-1)[order]
    counts = jnp.bincount(flat_e, length=N_EXPERTS).astype(jnp.int32)
    starts = jnp.cumsum(counts) - counts
    padded = (counts + MOE_BLOCK - 1) // MOE_BLOCK * MOE_BLOCK
    pad_ends = jnp.cumsum(padded)
    pad_starts = pad_ends - padded
    dest = pad_starts[sorted_e] + (jnp.arange(TK, dtype=jnp.int32) - starts[sorted_e])
    n_blocks = -(-TK // MOE_BLOCK) + N_EXPERTS
    P = n_blocks * MOE_BLOCK
    row_tok = jnp.full((P,), T, dtype=jnp.int32).at[dest].set(sorted_tok)
    h_pad = jnp.concatenate([h, jnp.zeros((1, D), h.dtype)], axis=0)
    xb = h_pad[row_tok].reshape(n_blocks, MOE_BLOCK, D)
    block_e = jnp.minimum(jnp.searchsorted(pad_ends, jnp.arange(n_blocks, dtype=jnp.int32) * MOE_BLOCK, side='right'), N_EXPERTS - 1)

    def expert_block(args):
        xblk, e = args
        gu = xblk @ w_gate_up[e] + b_gate_up[e]
        glu = jnp.minimum(gu[:, :D_EXPERT], SWIGLU_LIMIT)
        lin = jnp.clip(gu[:, D_EXPERT:], -SWIGLU_LIMIT, SWIGLU_LIMIT)
        act = glu * jax.nn.sigmoid(SWIGLU_ALPHA * glu) * (lin + 1.0)
        return act @ w_down[e] + b_down[e]

    yb = lax.map(expert_block, (xb, block_e)).reshape(P, D)
    y_assign = yb[dest] * sorted_gate[:, None].astype(yb.dtype)
    return jax.ops.segment_sum(y_assign, sorted_tok, num_segments=T)


def setup_inputs(seed: int = 0) -> dict:
    key = jax.random.key(seed)
    ks = iter(jax.random.split(key, 32))
    L, D = DEPTH, D_MODEL

    def nrm(shape, scale):
        return jax.random.normal(next(ks), shape, jnp.float32) * scale

    u = jax.random.uniform(next(ks), (L, 2, SSD_HEADS), jnp.float32)
    dt0 = jnp.exp(u * (math.log(0.1) - math.log(0.001)) + math.log(0.001))
    ssd_dt_bias = dt0 + jnp.log(-jnp.expm1(-dt0))
    ssd_a_log = jnp.log(jax.random.uniform(next(ks), (L, 2, SSD_HEADS), jnp.float32, minval=1.0, maxval=16.0))
    return {
        "x": nrm((BATCH, SEQ, D), 1.0),
        "c": nrm((BATCH, D), 1.0),
        "ctx": nrm((BATCH, CTX_LEN, D), 1.0),
        "c_ctx": nrm((D,), 1.0),
        "w_ada": nrm((L, D, 6 * D), 0.5 * D ** -0.5),
        "b_ada": nrm((L, 6 * D), 0.01),
        "norm1_w": 1.0 + nrm((L, D), 0.02),
        "w_in": nrm((L, D, IN_WIDTH), D ** -0.5),
        "da_lambda": nrm((L, 4, DA_QK), 0.1),
        "da_subln_w": 1.0 + nrm((L, DA_V), 0.02),
        "ssd_conv_w": nrm((L, SSD_CONV, SSD_CONV_CH), SSD_CONV ** -0.5),
        "ssd_conv_b": nrm((L, SSD_CONV_CH), 0.01),
        "ssd_a_log": ssd_a_log,
        "ssd_dt_bias": ssd_dt_bias,
        "ssd_d": 1.0 + nrm((L, SSD_HEADS), 0.02),
        "ssd_norm_w": 1.0 + nrm((L, SSD_WIDTH), 0.02),
        "gla_gk_up": nrm((L, 2, GLA_RANK, GLA_KEY_WIDTH), GLA_RANK ** -0.5),
        "gla_gk_b": nrm((L, 2, GLA_KEY_WIDTH), 0.01),
        "gla_norm_w": 1.0 + nrm((L, GLA_DV), 0.02),
        "w_out": nrm((L, MIX_WIDTH, D), MIX_WIDTH ** -0.5),
        "norm2_w": 1.0 + nrm((L, D), 0.02),
        "w_router": nrm((L, D, N_EXPERTS), D ** -0.5),
        "b_router": nrm((L, N_EXPERTS), 0.01),
        "w_gate_up": nrm((L, N_EXPERTS, D, 2 * D_EXPERT), D ** -0.5),
        "b_gate_up": nrm((L, N_EXPERTS, 2 * D_EXPERT), 0.01),
        "w_down": nrm((L, N_EXPERTS, D_EXPERT, D), D_EXPERT ** -0.5),
        "b_down": nrm((L, N_EXPERTS, D), 0.01),
        "final_norm_w": 1.0 + nrm((D,), 0.02),
    }


def reference(x, c, ctx, c_ctx, w_ada, b_ada, norm1_w, w_in, da_lambda, da_subln_w,
              ssd_conv_w, ssd_conv_b, ssd_a_log, ssd_dt_bias, ssd_d, ssd_norm_w,
              gla_gk_up, gla_gk_b, gla_norm_w, w_out, norm2_w, w_router, b_router,
              w_gate_up, b_gate_up, w_down, b_down, final_norm_w):
    f32 = jnp.float32
    Bsz, S, D = x.shape
    Lc = ctx.shape[1]
    ROWS = S // GRID_W
    row = jnp.repeat(jnp.arange(ROWS, dtype=jnp.int32), GRID_W)
    col = jnp.arange(S, dtype=jnp.int32) % GRID_W
    zero_ssd = jnp.zeros((Bsz, SSD_HEADS, SSD_HEAD_DIM, SSD_STATE), f32)
    zero_gla = jnp.zeros((Bsz, GLA_HEADS, GLA_DK, GLA_DV), f32)

    for l in range(DEPTH):
        last = l == DEPTH - 1
        lambda_init = 0.8 - 0.6 * math.exp(-0.3 * l)
        mod = jax.nn.silu(c) @ w_ada[l] + b_ada[l]
        mod_c = jax.nn.silu(c_ctx) @ w_ada[l] + b_ada[l]
        sh1, sc1, g1, sh2, sc2, g2 = [m[:, None, :] for m in jnp.split(mod, 6, axis=-1)]
        sh1c, sc1c, g1c, sh2c, sc2c, g2c = jnp.split(mod_c, 6)

        h = rms_norm(x, norm1_w[l]) * (1.0 + sc1) + sh1
        hc = rms_norm(ctx, norm1_w[l]) * (1.0 + sc1c) + sh1c
        pl = split_cols(h @ w_in[l])
        pc = split_cols(hc @ w_in[l])

        lp = da_lambda[l].astype(f32)
        lam = jnp.exp(jnp.sum(lp[0] * lp[1])) - jnp.exp(jnp.sum(lp[2] * lp[3])) + lambda_init
        q_l = axial_rope(pl[0].reshape(Bsz, S, DA_HEADS, 2, DA_QK), row, col)
        k_l = axial_rope(pl[1].reshape(Bsz, S, DA_HEADS, 2, DA_QK), row, col)
        v_l = pl[2].reshape(Bsz, S, DA_HEADS, DA_V)
        q_c = pc[0].reshape(Bsz, Lc, DA_HEADS, 2, DA_QK)
        k_c = pc[1].reshape(Bsz, Lc, DA_HEADS, 2, DA_QK)
        v_c = pc[2].reshape(Bsz, Lc, DA_HEADS, DA_V)
        k_all = jnp.concatenate([k_c, k_l], axis=1)
        v_all = jnp.concatenate([v_c, v_l], axis=1)
        o_a = diff_attention_latent(q_l, k_all, v_all, lam)
        a_l = (rms_norm(o_a, da_subln_w[l]) * (1.0 - lambda_init)).reshape(Bsz, S, DA_WIDTH).astype(x.dtype)

        y_c, st_c = ssd_sequence(pc[4], pc[5], ssd_conv_w[l], ssd_conv_b[l], ssd_a_log[l],
                                 ssd_dt_bias[l], ssd_d[l], (zero_ssd, zero_ssd))
        y_l, _ = ssd_sequence(pl[4], pl[5], ssd_conv_w[l], ssd_conv_b[l], ssd_a_log[l],
                              ssd_dt_bias[l], ssd_d[l], st_c)
        s_l = ssd_gated_norm(y_l, pl[3], ssd_norm_w[l]).astype(x.dtype)

        o_gc, gs_c = gla_sequence(pc[6], pc[7], pc[8], pc[10], gla_gk_up[l], gla_gk_b[l], (zero_gla, zero_gla))
        o_gl, _ = gla_sequence(pl[6], pl[7], pl[8], pl[10], gla_gk_up[l], gla_gk_b[l], gs_c)
        c_l = gla_finish(o_gl, pl[9], gla_norm_w[l]).astype(x.dtype)

        mix_l = jnp.concatenate([a_l, s_l, c_l], axis=-1)
        x = x + g1 * (mix_l @ w_out[l])

        if not last:
            o_ac = diff_softmax_attend(q_c, k_c, v_c, lam)
            a_c = (rms_norm(o_ac, da_subln_w[l]) * (1.0 - lambda_init)).reshape(Bsz, Lc, DA_WIDTH).astype(ctx.dtype)
            s_c = ssd_gated_norm(y_c, pc[3], ssd_norm_w[l]).astype(ctx.dtype)
            c_c = gla_finish(o_gc, pc[9], gla_norm_w[l]).astype(ctx.dtype)
            mix_c = jnp.concatenate([a_c, s_c, c_c], axis=-1)
            ctx = ctx + g1c * (mix_c @ w_out[l])

            h2 = rms_norm(x, norm2_w[l]) * (1.0 + sc2) + sh2
            h2c = rms_norm(ctx, norm2_w[l]) * (1.0 + sc2c) + sh2c
            tokens = jnp.concatenate([h2.reshape(Bsz * S, D), h2c.reshape(Bsz * Lc, D)], axis=0)
            ff = moe_ffn(tokens, w_router[l], b_router[l], w_gate_up[l], b_gate_up[l], w_down[l], b_down[l])
            x = x + g2 * ff[:Bsz * S].reshape(Bsz, S, D)
            ctx = ctx + g2c * ff[Bsz * S:].reshape(Bsz, Lc, D)
        else:
            h2 = rms_norm(x, norm2_w[l]) * (1.0 + sc2) + sh2
            ff = moe_ffn(h2.reshape(Bsz * S, D), w_router[l], b_router[l], w_gate_up[l], b_gate_up[l], w_down[l], b_down[l])
            x = x + g2 * ff.reshape(Bsz, S, D)

    return rms_norm(x, final_norm_w)
```

```python
import math
import numpy as np
import concourse.bass as bass
import concourse.mybir as mybir
from concourse.bass_utils import run_bass_kernel_spmd

F32 = mybir.dt.float32
BF16 = mybir.dt.bfloat16
ALU = mybir.AluOpType
AF = mybir.ActivationFunctionType
AX = mybir.AxisListType

COMPUTE = ("pe", "act", "dve", "pool")
NCORES = 8
D = 1024
EPS = 1e-6


class Prog:
    def __init__(self, nc, n_dma_sems=8, same_engine_sync=True):
        self.nc = nc
        self.same_engine_sync = same_engine_sync
        self.streams = {e: [] for e in ("pe", "act", "dve", "pool", "sp")}
        self.sem = {e: nc.alloc_semaphore(f"cs_{e}") for e in COMPUTE}
        self.cnt = {e: 0 for e in COMPUTE}
        self.seen = {e: {} for e in self.streams}
        self.dma_sems = {}
        self.dma_tot = {}
        self.dma_rr = {}
        for q in ("sp", "act", "pool"):
            n = n_dma_sems if q != "act" else 4
            self.dma_sems[q] = [nc.alloc_semaphore(f"ds_{q}{i}") for i in range(n)]
            self.dma_rr[q] = 0
            for s in self.dma_sems[q]:
                self.dma_tot[id(s)] = 0
        self.res = {}
        self.semobj = {}

    def _deps(self, reads, writes):
        deps = []
        for r in reads:
            st = self.res.get(r)
            if st is not None and st[0] is not None:
                deps.append(st[0])
        for w in writes:
            st = self.res.get(w)
            if st is not None:
                if st[0] is not None:
                    deps.append(st[0])
                deps.extend(st[1])
        return deps

    def _commit(self, reads, writes, token):
        for r in reads:
            st = self.res.setdefault(r, [None, []])
            st[1].append(token)
        for w in writes:
            self.res[w] = [token, []]

    def _emit(self, eng, deps, fn, token, own_sem=None):
        waits = []
        seen = self.seen[eng]
        best = {}
        for (s, v) in deps:
            k = id(s)
            self.semobj[k] = s
            if own_sem is not None and s is own_sem and (eng == "pe" or not self.same_engine_sync):
                continue
            if v > best.get(k, 0):
                best[k] = v
        for k, v in best.items():
            if v > seen.get(k, 0):
                seen[k] = v
                waits.append((self.semobj[k], v))
        self.streams[eng].append((waits, fn, token))

    def op(self, eng, fn, reads=(), writes=()):
        deps = self._deps(reads, writes)
        self.cnt[eng] += 1
        token = (self.sem[eng], self.cnt[eng])
        self._emit(eng, deps, fn, (self.sem[eng], 1), own_sem=self.sem[eng])
        self._commit(reads, writes, token)
        return token

    def dma(self, q, out, in_, reads=(), writes=(), **kw):
        deps = self._deps(reads, writes)
        i = self.dma_rr[q]
        self.dma_rr[q] = (i + 1) % len(self.dma_sems[q])
        s = self.dma_sems[q][i]
        tot = self.dma_tot[id(s)]
        if tot > 0:
            deps.append((s, tot))
        self.dma_tot[id(s)] = tot + 16
        token = (s, tot + 16)

        def fn(e, out=out, in_=in_, kw=kw):
            return e.dma_start(out=out, in_=in_, **kw)

        self._emit(q, deps, fn, (s, 16))
        self._commit(reads, writes, token)
        return token

    def finish(self):
        deps = []
        for e in COMPUTE:
            if self.cnt[e] > 0:
                deps.append((self.sem[e], self.cnt[e]))
        for q in self.dma_sems:
            for s in self.dma_sems[q]:
                if self.dma_tot[id(s)] > 0:
                    deps.append((s, self.dma_tot[id(s)]))
        self._emit("sp", deps, None, None)

    def emit(self):
        nc = self.nc
        with nc.Block() as block:
            def run(eng_name):
                def body(e):
                    for waits, fn, token in self.streams[eng_name]:
                        for (s, v) in waits:
                            e.wait_ge(s, v)
                        if fn is not None:
                            fn(e).then_inc(token[0], token[1])
                return body
            block.sync(run("sp"))
            block.tensor(run("pe"))
            block.scalar(run("act"))
            block.vector(run("dve"))
            block.gpsimd(run("pool"))


def _din(nc, name, shape, dt=F32):
    return nc.dram_tensor(name, list(shape), dt, kind="ExternalInput").ap()


def _dout(nc, name, shape, dt=F32):
    return nc.dram_tensor(name, list(shape), dt, kind="ExternalOutput").ap()


ACOLS = 1536


def build_A():
    nc = bass.Bass("TRN2", target_bir_lowering=False)
    cT = _din(nc, "cT", [128, 8, 3])
    wa = _din(nc, "wa", [1024, ACOLS])
    ba = _din(nc, "ba", [1, ACOLS])
    modp = _dout(nc, "modp", [3, ACOLS])
    P = Prog(nc)
    cs = nc.alloc_sbuf_tensor("cs", [128, 8, 3], F32)
    sc = nc.alloc_sbuf_tensor("sc", [128, 8, 3], F32)
    was = nc.alloc_sbuf_tensor("was", [128, 8, ACOLS], F32)
    bas = nc.alloc_sbuf_tensor("bas", [1, ACOLS], F32)
    ones = nc.alloc_sbuf_tensor("ones", [1, 4], F32)
    om = nc.alloc_sbuf_tensor("om", [3, ACOLS], F32)
    P.dma("sp", cs[:], cT, writes=["cs"])
    for k in range(8):
        P.dma("sp" if k % 2 == 0 else "pool", was[:, k, :], wa[k * 128:(k + 1) * 128, :], writes=[("was", k)])
    P.dma("sp", bas[:], ba, writes=["bas"])
    P.op("dve", lambda e: e.memset(ones[:], 1.0), writes=["ones"])
    P.op("act", lambda e: e.activation(out=sc[:], in_=cs[:], func=AF.Silu), reads=["cs"], writes=["sc"])
    for nb in range(ACOLS // 512):
        ps = nc.alloc_psum_tensor(f"psA{nb}", [3, 512], F32)
        sl = slice(nb * 512, (nb + 1) * 512)

        def mm(e, ps=ps, sl=sl):
            for k in range(8):
                e.matmul(ps[:], sc[:, k, :], was[:, k, sl], start=(k == 0), stop=False)
            return e.matmul(ps[:], ones[0:1, 0:3], bas[0:1, sl], start=False, stop=True)
        P.op("pe", mm, reads=["sc", "bas", "ones"] + [("was", k) for k in range(8)], writes=[("psA", nb)])
        P.op("act", lambda e, ps=ps, sl=sl: e.copy(out=om[:, sl], in_=ps[:]), reads=[("psA", nb)], writes=[("om", nb)])
    P.dma("sp", modp, om[:], reads=[("om", nb) for nb in range(ACOLS // 512)], writes=["modp"])
    P.finish()
    P.emit()
    return nc


NTI = 17
INW = 3112


def build_P():
    nc = bass.Bass("TRN2", target_bir_lowering=False)
    xs = _din(nc, "xs", [NTI, 128, D])
    ml = _din(nc, "ml", [2, D])
    mc = _din(nc, "mc", [2, D])
    n1w = _din(nc, "n1w", [1, D])
    w_in = _din(nc, "w_in", [D, INW])
    rc = _din(nc, "rc", [16, 128, 64])
    rs = _din(nc, "rs", [16, 128, 64])
    idn = _din(nc, "idn", [128, 128])
    proj = _dout(nc, "proj", [NTI, 128, INW])
    P = Prog(nc)
    W = nc.alloc_sbuf_tensor("W", [128, 8, INW], BF16)
    idb = nc.alloc_sbuf_tensor("idb", [128, 128], BF16)
    nwb = nc.alloc_sbuf_tensor("nwb", [128, D], F32)
    scb = nc.alloc_sbuf_tensor("scb", [128, D], F32)
    G = [nc.alloc_sbuf_tensor(f"G{j}", [128, D], F32) for j in range(2)]
    SH = [nc.alloc_sbuf_tensor(f"SH{j}", [128, D], F32) for j in range(2)]
    xts = [nc.alloc_sbuf_tensor(f"xt{j}", [128, D], F32) for j in range(2)]
    junk = nc.alloc_sbuf_tensor("junk", [128, D], F32)
    hf = nc.alloc_sbuf_tensor("hf", [128, D], F32)
    hb = nc.alloc_sbuf_tensor("hb", [128, D], BF16)
    hT = nc.alloc_sbuf_tensor("hT", [128, 8, 128], BF16)
    outs = [nc.alloc_sbuf_tensor(f"outt{j}", [128, INW], F32) for j in range(2)]
    tmp = nc.alloc_sbuf_tensor("tmp", [128, D], F32)
    rcs = [nc.alloc_sbuf_tensor(f"rc{j}", [128, 64], F32) for j in range(2)]
    rss = [nc.alloc_sbuf_tensor(f"rs{j}", [128, 64], F32) for j in range(2)]
    ss = nc.alloc_sbuf_tensor("ss", [128, NTI], F32)
    rstd = nc.alloc_sbuf_tensor("rstd", [128, NTI], F32)
    pst = nc.alloc_psum_tensor("pst", [128, 8, 128], BF16)
    pss = [nc.alloc_psum_tensor(f"psP{j}", [128, 512], F32) for j in range(3)]

    for k in range(8):
        P.dma("pool", W[:, k, :], w_in[k * 128:(k + 1) * 128, :], writes=[("W", k)])
    P.dma("pool", idb[:], idn, writes=["idb"])
    P.dma("sp", nwb[:], n1w[0:1, :].partition_broadcast(128), writes=["nwb"])
    for j, m in enumerate((ml, mc)):
        P.dma("sp", SH[j][:], m[0:1, :].partition_broadcast(128), writes=[f"SH{j}"])
        P.dma("sp", scb[:], m[1:2, :].partition_broadcast(128), reads=[], writes=["scb"])
        P.op("dve", lambda e, j=j: e.scalar_tensor_tensor(out=G[j][:], in0=scb[:], scalar=1.0, in1=nwb[:], op0=ALU.add, op1=ALU.mult),
             reads=["scb", "nwb"], writes=[f"G{j}"])
    Wall = [("W", k) for k in range(8)]
    cblocks = [(c0, min(c0 + 512, INW)) for c0 in range(0, INW, 512)]
    pi = 0
    for i in range(NTI):
        j = 0 if i < 16 else 1
        xt = xts[i % 2]
        xn = f"xt{i % 2}"
        outt = outs[i % 2]
        on = f"outt{i % 2}"
        P.dma("sp", xt[:], xs[i], writes=[xn])
        if i < 16:
            P.dma("sp", rcs[i % 2][:], rc[i], writes=[f"rc{i % 2}"])
            P.dma("sp", rss[i % 2][:], rs[i], writes=[f"rs{i % 2}"])
        P.op("act", lambda e, xt=xt, i=i: e.activation(out=junk[:], in_=xt[:], func=AF.Square, accum_out=ss[:, i:i + 1]),
             reads=[xn], writes=["junk", ("ss", i)])
        P.op("dve", lambda e, i=i: e.tensor_scalar(out=rstd[:, i:i + 1], in0=ss[:, i:i + 1], scalar1=1.0 / D, scalar2=EPS, op0=ALU.mult, op1=ALU.add),
             reads=[("ss", i)], writes=[("rstd", i)])
        P.op("act", lambda e, i=i: e.activation(out=rstd[:, i:i + 1], in_=rstd[:, i:i + 1], func=AF.Sqrt), reads=[("rstd", i)], writes=[("rstd", i)])
        P.op("dve", lambda e, i=i: e.reciprocal(out=rstd[:, i:i + 1], in_=rstd[:, i:i + 1]), reads=[("rstd", i)], writes=[("rstd", i)])
        P.op("dve", lambda e, xt=xt, i=i, j=j: e.scalar_tensor_tensor(out=hf[:], in0=xt[:], scalar=rstd[:, i:i + 1], in1=G[j][:], op0=ALU.mult, op1=ALU.mult),
             reads=[xn, ("rstd", i), f"G{j}"], writes=["hf"])
        P.op("pool", lambda e, j=j: e.tensor_tensor(out=hb[:], in0=hf[:], in1=SH[j][:], op=ALU.add), reads=["hf", f"SH{j}"], writes=["hb"])

        def tr(e):
            for k in range(8):
                ins = e.transpose(pst[:, k, :], hb[:, k * 128:(k + 1) * 128], idb[:])
            return ins
        P.op("pe", tr, reads=["hb", "idb"], writes=["pst"])
        P.op("act", lambda e: e.copy(out=hT[:], in_=pst[:]), reads=["pst"], writes=["hT"])
        for cb, (c0, c1) in enumerate(cblocks):
            ps = pss[pi % 3]
            pn = f"psP{pi % 3}"
            pi += 1

            def mm(e, ps=ps, c0=c0, c1=c1):
                for k in range(8):
                    ins = e.matmul(ps[:, 0:c1 - c0], hT[:, k, :], W[:, k, c0:c1], start=(k == 0), stop=(k == 7))
                return ins
            P.op("pe", mm, reads=["hT"] + Wall, writes=[pn])
            if cb % 2 == 0:
                P.op("act", lambda e, ps=ps, c0=c0, c1=c1, outt=outt: e.copy(out=outt[:, c0:c1], in_=ps[:, 0:c1 - c0]), reads=[pn], writes=[(on, cb)])
            else:
                P.op("dve", lambda e, ps=ps, c0=c0, c1=c1, outt=outt: e.tensor_copy(out=outt[:, c0:c1], in_=ps[:, 0:c1 - c0]), reads=[pn], writes=[(on, cb)])
        if i < 16:
            rcn, rsn = f"rc{i % 2}", f"rs{i % 2}"
            rct, rst = rcs[i % 2], rss[i % 2]
            ov = outt[:, 0:1024].rearrange("p (g q d) -> p g q d", g=16, q=4)
            tv = tmp[:].rearrange("p (g q d) -> p g q d", g=16, q=4)
            for q in range(4):
                qs = q ^ 1
                P.op("dve", lambda e, q=q, qs=qs, ov=ov, tv=tv, rst=rst: e.tensor_tensor(
                    out=tv[:, :, q, :], in0=ov[:, :, qs, :], in1=rst[:, q * 16:(q + 1) * 16].unsqueeze(1).broadcast_to([128, 16, 16]), op=ALU.mult),
                    reads=[(on, 0), (on, 1), rsn], writes=[("tmp", q)])
            o3 = outt[:, 0:1024].rearrange("p (g d) -> p g d", g=16)
            P.op("pool", lambda e, o3=o3, rct=rct: e.tensor_tensor(out=o3, in0=o3, in1=rct[:].unsqueeze(1).broadcast_to([128, 16, 64]), op=ALU.mult),
                 reads=[(on, 0), (on, 1), rcn] + [("tmp", q) for q in range(4)], writes=[(on, 0), (on, 1)])
            P.op("dve", lambda e, outt=outt: e.tensor_tensor(out=outt[:, 0:1024], in0=outt[:, 0:1024], in1=tmp[:], op=ALU.add),
                 reads=[(on, 0), (on, 1)] + [("tmp", q) for q in range(4)], writes=[(on, 0), (on, 1)])
        P.dma("sp", proj[i], outt[:], reads=[(on, cb) for cb in range(len(cblocks))], writes=[("proj", i)])
    P.finish()
    P.emit()
    return nc


TT = 8448
NKT = 66
LC = 256


def build_MA(lambda_init):
    nc = bass.Bass("TRN2", target_bir_lowering=False)
    QT = _din(nc, "QT", [128, TT])
    KT = _din(nc, "KT", [128, TT])
    V = _din(nc, "V", [NKT, 128, 128])
    lp = _din(nc, "lp", [1, 256])
    sw = _din(nc, "sw", [1, 128])
    idn = _din(nc, "idn", [128, 128])
    aout = _dout(nc, "aout", [NKT, 128, 128])
    P = Prog(nc)
    scale = 64 ** -0.5
    QTb = nc.alloc_sbuf_tensor("QTb", [128, TT], BF16)
    KTb = nc.alloc_sbuf_tensor("KTb", [128, TT], BF16)
    Va = nc.alloc_sbuf_tensor("Va", [128, NKT, 130], BF16)
    sq = nc.alloc_sbuf_tensor("sq", [128, TT], F32)
    sq2 = nc.alloc_sbuf_tensor("sq2", [128, TT], F32)
    bones = nc.alloc_sbuf_tensor("bones", [128, 2], F32)
    ident = nc.alloc_sbuf_tensor("ident", [128, 128], F32)
    nrm = nc.alloc_sbuf_tensor("nrm", [2, 2, TT], F32)
    mx = nc.alloc_sbuf_tensor("mx", [2, 2], F32)
    mprod = nc.alloc_sbuf_tensor("mprod", [2, 1], F32)
    dg = nc.alloc_sbuf_tensor("dg", [2, 2], F32)
    ones2 = nc.alloc_sbuf_tensor("ones2", [2, 128], F32)
    negM = nc.alloc_sbuf_tensor("negM", [128, 2], F32)
    lps = nc.alloc_sbuf_tensor("lps", [1, 256], F32)
    lpp = nc.alloc_sbuf_tensor("lpp", [1, 2, 64], F32)
    lsum = nc.alloc_sbuf_tensor("lsum", [1, 2], F32)
    lam1 = nc.alloc_sbuf_tensor("lam1", [1, 1], F32)
    nlam = nc.alloc_sbuf_tensor("nlam", [128, 1], F32)
    swb = nc.alloc_sbuf_tensor("swb", [128, 128], F32)
    Pts = [nc.alloc_sbuf_tensor(f"Pt{j}", [128, 512], BF16) for j in range(4)]
    osb = nc.alloc_sbuf_tensor("osb", [128, 2, 130], F32)
    rec = nc.alloc_sbuf_tensor("rec", [128, 2], F32)
    t2 = nc.alloc_sbuf_tensor("t2", [128, 1], F32)
    oa = nc.alloc_sbuf_tensor("oa", [128, 128], F32)
    od = nc.alloc_sbuf_tensor("od", [128, 128], F32)
    junk = nc.alloc_sbuf_tensor("junkA", [128, 128], F32)
    ssq = nc.alloc_sbuf_tensor("ssq", [128, 1], F32)
    ofin = [nc.alloc_sbuf_tensor(f"ofin{j}", [128, 128], F32) for j in range(2)]
    ps_s = [[nc.alloc_psum_tensor(f"pss{m}{j}", [128, 512], F32) for j in range(2)] for m in range(2)]
    acc = [[nc.alloc_psum_tensor(f"acc{m}{j}", [128, 2, 256], F32) for j in range(2)] for m in range(2)]

    P.dma("pool", QTb[:], QT, writes=["QTb"])
    P.dma("pool", KTb[:], KT, writes=["KTb"])
    P.dma("pool", Va[:, :, 0:128], V.rearrange("t p e -> p t e"), writes=["Va"])
    P.op("dve", lambda e: e.memset(Va[:, :, 128:130], 1.0), writes=["Va1"])
    P.dma("sp", sq[:], QT, writes=["sq"])
    P.dma("sp", sq2[:], KT, writes=["sq2"])
    P.dma("sp", ident[:], idn, writes=["ident"])
    P.dma("sp", lps[:], lp, writes=["lps"])
    P.dma("sp", swb[:], sw[0:1, :].partition_broadcast(128), writes=["swb"])
    P.op("dve", lambda e: e.memset(bones[:], 0.0), writes=["bones"])
    P.op("dve", lambda e: e.memset(bones[0:64, 0:1], 1.0), reads=["bones"], writes=["bones"])
    P.op("dve", lambda e: e.memset(bones[64:128, 1:2], 1.0), reads=["bones"], writes=["bones"])
    P.op("dve", lambda e: e.memset(ones2[:], 1.0), writes=["ones2"])
    P.op("act", lambda e: e.activation(out=sq[:], in_=sq[:], func=AF.Square), reads=["sq"], writes=["sq"])
    P.op("pool", lambda e: e.tensor_tensor(out=sq2[:], in0=sq2[:], in1=sq2[:], op=ALU.mult), reads=["sq2"], writes=["sq2"])
    pi = 0
    for which, src in enumerate((sq, sq2)):
        sn = "sq" if which == 0 else "sq2"
        for c0 in range(0, TT, 512):
            c1 = min(c0 + 512, TT)
            ps = ps_s[0][pi % 2]
            pn = f"pss0{pi % 2}"
            pi += 1
            P.op("pe", lambda e, ps=ps, src=src, c0=c0, c1=c1: e.matmul(ps[0:2, 0:c1 - c0], bones[:], src[:, c0:c1], start=True, stop=True),
                 reads=[sn, "bones"], writes=[pn])
            P.op("dve", lambda e, ps=ps, which=which, c0=c0, c1=c1: e.tensor_copy(out=nrm[:, which, c0:c1], in_=ps[0:2, 0:c1 - c0]),
                 reads=[pn], writes=[("nrm", which, c0)])
    nrm_all = [("nrm", w, c0) for w in range(2) for c0 in range(0, TT, 512)]
    P.op("dve", lambda e: e.tensor_reduce(out=mx[:], in_=nrm[:], axis=AX.X, op=ALU.max), reads=nrm_all, writes=["mx"])
    P.op("dve", lambda e: e.tensor_tensor(out=mprod[:], in0=mx[:, 0:1], in1=mx[:, 1:2], op=ALU.mult), reads=["mx"], writes=["mprod"])
    P.op("act", lambda e: e.activation(out=mprod[:], in_=mprod[:], func=AF.Sqrt), reads=["mprod"], writes=["mprod"])
    P.op("dve", lambda e: e.tensor_scalar(out=dg[:], in0=ident[0:2, 0:2], scalar1=mprod[:, 0:1], scalar2=-scale, op0=ALU.mult, op1=ALU.mult),
         reads=["mprod", "ident"], writes=["dg"])
    P.op("pe", lambda e: e.matmul(ps_s[1][0][:, 0:2], ones2[:], dg[:], start=True, stop=True), reads=["ones2", "dg"], writes=["pss10"])
    P.op("dve", lambda e: e.tensor_copy(out=negM[:], in_=ps_s[1][0][:, 0:2]), reads=["pss10"], writes=["negM"])
    P.op("dve", lambda e: e.tensor_tensor(out=lpp[:], in0=lps[:].rearrange("p (a b d) -> p a b d", a=2, b=2)[:, :, 0, :],
                                          in1=lps[:].rearrange("p (a b d) -> p a b d", a=2, b=2)[:, :, 1, :], op=ALU.mult), reads=["lps"], writes=["lpp"])
    P.op("dve", lambda e: e.tensor_reduce(out=lsum[:], in_=lpp[:], axis=AX.X, op=ALU.add), reads=["lpp"], writes=["lsum"])
    P.op("act", lambda e: e.activation(out=lsum[:], in_=lsum[:], func=AF.Exp), reads=["lsum"], writes=["lsum"])
    P.op("dve", lambda e: e.tensor_tensor(out=lam1[:], in0=lsum[:, 1:2], in1=lsum[:, 0:1], op=ALU.subtract), reads=["lsum"], writes=["lam1"])
    P.op("dve", lambda e: e.tensor_scalar(out=lam1[:], in0=lam1[:], scalar1=-float(lambda_init), scalar2=None, op0=ALU.add), reads=["lam1"], writes=["lam1"])
    P.op("pe", lambda e: e.matmul(ps_s[1][1][:, 0:1], ones2[0:1, :], lam1[:], start=True, stop=True), reads=["ones2", "lam1"], writes=["pss11"])
    P.op("dve", lambda e: e.tensor_copy(out=nlam[:], in_=ps_s[1][1][:, 0:1]), reads=["pss11"], writes=["nlam"])
    P.op("dve", lambda e: e.tensor_scalar(out=swb[:], in0=swb[:], scalar1=float(1.0 - lambda_init), scalar2=None, op0=ALU.mult), reads=["swb"], writes=["swb"])

    blocks = [(0, 256, 2)] + [(LC + 512 * i, 512, NKT) for i in range(16)]
    pti = 0
    oi = 0
    for (q0, qn, nk) in blocks:
        nsb = qn // 128
        for kt in range(nk):
            for m in range(2):
                ps = ps_s[m][kt % 2]
                pn = f"pss{m}{kt % 2}"
                P.op("pe", lambda e, ps=ps, m=m, kt=kt, q0=q0, qn=qn: e.matmul(
                    ps[:, 0:qn], KTb[m * 64:(m + 1) * 64, kt * 128:(kt + 1) * 128], QTb[m * 64:(m + 1) * 64, q0:q0 + qn], start=True, stop=True),
                    reads=["QTb", "KTb"], writes=[pn])
                Pt = Pts[pti % 4]
                ptn = f"Pt{pti % 4}"
                pti += 1
                P.op("act", lambda e, ps=ps, Pt=Pt, m=m, qn=qn: e.activation(out=Pt[:, 0:qn], in_=ps[:, 0:qn], func=AF.Exp, bias=negM[:, m:m + 1], scale=scale),
                     reads=[pn, "negM"], writes=[ptn])

                def pv(e, Pt=Pt, m=m, kt=kt, nsb=nsb, nk=nk):
                    for sb in range(nsb):
                        ins = e.matmul(acc[m][sb // 2][:, sb % 2, 0:129], Pt[:, sb * 128:(sb + 1) * 128], Va[:, kt, 0:129], start=(kt == 0 and sb % 2 == 0), stop=(kt == nk - 1), skip_group_check=True)
                    return ins
                P.op("pe", pv, reads=[ptn, "Va", "Va1"], writes=[f"acc{m}"])
        for sb in range(nsb):
            for m in range(2):
                if m == 0:
                    P.op("act", lambda e, m=m, sb=sb: e.copy(out=osb[:, m, 0:129], in_=acc[m][sb // 2][:, sb % 2, 0:129]), reads=[f"acc{m}"], writes=[("osb", m)])
                else:
                    P.op("dve", lambda e, m=m, sb=sb: e.tensor_copy(out=osb[:, m, 0:129], in_=acc[m][sb // 2][:, sb % 2, 0:129]), reads=[f"acc{m}"], writes=[("osb", m)])
            P.op("dve", lambda e: e.reciprocal(out=rec[:], in_=osb[:, :, 128]), reads=[("osb", 0), ("osb", 1)], writes=["rec"])
            P.op("dve", lambda e: e.tensor_tensor(out=t2[:], in0=rec[:, 1:2], in1=nlam[:], op=ALU.mult), reads=["rec", "nlam"], writes=["t2"])
            P.op("dve", lambda e: e.tensor_scalar(out=oa[:], in0=osb[:, 0, 0:128], scalar1=rec[:, 0:1], scalar2=None, op0=ALU.mult), reads=[("osb", 0), "rec"], writes=["oa"])
            P.op("dve", lambda e: e.scalar_tensor_tensor(out=od[:], in0=osb[:, 1, 0:128], scalar=t2[:, 0:1], in1=oa[:], op0=ALU.mult, op1=ALU.add),
                 reads=[("osb", 1), "t2", "oa"], writes=["od"])
            P.op("act", lambda e: e.activation(out=junk[:], in_=od[:], func=AF.Square, accum_out=ssq[:]), reads=["od"], writes=["junkA", "ssq"])
            P.op("dve", lambda e: e.tensor_scalar(out=ssq[:], in0=ssq[:], scalar1=1.0 / 128, scalar2=EPS, op0=ALU.mult, op1=ALU.add), reads=["ssq"], writes=["ssq"])
            P.op("act", lambda e: e.activation(out=ssq[:], in_=ssq[:], func=AF.Sqrt), reads=["ssq"], writes=["ssq"])
            P.op("dve", lambda e: e.reciprocal(out=ssq[:], in_=ssq[:]), reads=["ssq"], writes=["ssq"])
            of = ofin[oi % 2]
            ofn = f"ofin{oi % 2}"
            oi += 1
            P.op("dve", lambda e, of=of: e.scalar_tensor_tensor(out=of[:], in0=od[:], scalar=ssq[:, 0:1], in1=swb[:], op0=ALU.mult, op1=ALU.mult),
                 reads=["od", "ssq", "swb"], writes=[ofn])
            P.dma("sp", aout[(q0 // 128) + sb], of[:], reads=[ofn], writes=[("aout", q0, sb)])
    P.finish()
    P.emit()
    return nc


def _conv_silu(P, nc, np_, src_dram, cw, cb, stage_in, stage_acc, out_bf, tag):
    si, sa = stage_in, stage_acc
    P.dma("sp", si[0:np_, :], src_dram, writes=["stage_in"])
    P.op("dve", lambda e: e.tensor_scalar(out=sa[0:np_, :], in0=si[0:np_, :], scalar1=cw[0:np_, 1:2], scalar2=cb[0:np_, 0:1], op0=ALU.mult, op1=ALU.add),
         reads=["stage_in", tag + "cw", tag + "cb"], writes=["stage_acc"])
    for (lo, hi) in ((0, LC), (LC, TT)):
        P.op("dve", lambda e, lo=lo, hi=hi: e.scalar_tensor_tensor(out=sa[0:np_, lo + 1:hi], in0=si[0:np_, lo:hi - 1], scalar=cw[0:np_, 0:1], in1=sa[0:np_, lo + 1:hi], op0=ALU.mult, op1=ALU.add),
             reads=["stage_in", tag + "cw", "stage_acc"], writes=["stage_acc"])
        P.op("dve", lambda e, lo=lo, hi=hi: e.scalar_tensor_tensor(out=sa[0:np_, lo:hi - 1], in0=si[0:np_, lo + 1:hi], scalar=cw[0:np_, 2:3], in1=sa[0:np_, lo:hi - 1], op0=ALU.mult, op1=ALU.add),
             reads=["stage_in", tag + "cw", "stage_acc"], writes=["stage_acc"])
    P.op("act", lambda e: e.activation(out=out_bf[0:np_, :], in_=sa[0:np_, :], func=AF.Silu), reads=["stage_acc"], writes=[tag + "T"])


def build_MS():
    nc = bass.Bass("TRN2", target_bir_lowering=False)
    xTp = _din(nc, "xTp", [128, TT])
    BTp = _din(nc, "BTp", [64, TT])
    CTp = _din(nc, "CTp", [64, TT])
    cwx = _din(nc, "cwx", [128, 3]); cbx = _din(nc, "cbx", [128, 1])
    cwB = _din(nc, "cwB", [64, 3]); cbB = _din(nc, "cbB", [64, 1])
    cwC = _din(nc, "cwC", [64, 3]); cbC = _din(nc, "cbC", [64, 1])
    zin = _din(nc, "z", [NKT, 128, 128])
    dtr = _din(nc, "dtr", [NKT, 128, 4])
    dtb = _din(nc, "dtb", [1, 4])
    alog = _din(nc, "alog", [1, 4])
    dsk = _din(nc, "dsk", [1, 128])
    nw = _din(nc, "nw", [1, 128])
    Uin = _din(nc, "U", [128, 128])
    Lin = _din(nc, "L", [128, 128])
    idn = _din(nc, "idn", [128, 128])
    sout = _dout(nc, "sout", [NKT, 128, 128])
    P = Prog(nc)
    A = nc.alloc_sbuf_tensor
    stage_in = A("stage_in", [128, TT], F32)
    stage_acc = A("stage_acc", [128, TT], F32)
    xT = A("xT", [128, TT], BF16)
    BT = A("BT", [64, TT], BF16)
    CT = A("CT", [64, TT], BF16)
    x_tok = A("x_tok", [128, NKT, 128], BF16)
    B_tok = A("B_tok", [128, NKT, 64], BF16)
    yacc = A("yacc", [128, NKT, 128], F32)
    cw = {"x": A("cwx_s", [128, 3], F32), "B": A("cwB_s", [64, 3], F32), "C": A("cwC_s", [64, 3], F32)}
    cb = {"x": A("cbx_s", [128, 1], F32), "B": A("cbB_s", [64, 1], F32), "C": A("cbC_s", [64, 1], F32)}
    dt = A("dt", [128, NKT, 4], F32)
    av = A("av", [128, NKT, 4], F32)
    dtb_s = A("dtb_s", [128, 4], F32)
    A_s = A("A_s", [128, 4], F32)
    dsk_s = A("dsk_s", [128, 128], F32)
    nw_s = A("nw_s", [128, 128], F32)
    TRI = [A("U_s", [128, 128], F32), A("L_s", [128, 128], F32)]
    idb = A("idbS", [128, 128], BF16)
    ones = A("onesS", [128, 128], F32)
    Abc = [A(f"Abc{j}", [128, 128], F32) for j in range(2)]
    acsc = A("acsc", [128, 2], F32)
    sg = [A(f"sg{j}", [128, 128], F32) for j in range(2)]
    dec = [A(f"dec{j}", [128, 128], F32) for j in range(2)]
    t1 = [A(f"t1{j}", [128, 128], F32) for j in range(2)]
    scb = [A(f"scbf{j}", [128, 128], BF16) for j in range(2)]
    ea = [A(f"ea{j}", [64, 128], F32) for j in range(2)]
    Cp = [A(f"Cp{j}", [64, 128], BF16) for j in range(2)]
    wl = A("wl", [128, 2], F32)
    xw = [A(f"xw{j}", [128, 64], BF16) for j in range(2)]
    cd = A("cd", [64, 2], F32)
    hst = [[A(f"hst{d}{j}", [64, 64], F32) for j in range(2)] for d in range(2)]
    hstb = [[A(f"hstb{d}{j}", [64, 64], BF16) for j in range(2)] for d in range(2)]
    tmpy = A("tmpy", [128, 128], F32)
    rs_ = A("rsS", [128, NKT], F32)
    PS = nc.alloc_psum_tensor
    p_row = [PS(f"p_row{j}", [128, 512], F32) for j in range(2)]
    p_col = PS("p_col", [128, 512], F32)
    p_cb = PS("p_cb", [128, 512], F32)
    p_y = PS("p_y", [128, 512], F32)
    p_st = PS("p_st", [128, 512], F32)
    p_tr = PS("p_tr", [128, 8, 128], BF16)

    for k, (w_, b_) in {"x": (cwx, cbx), "B": (cwB, cbB), "C": (cwC, cbC)}.items():
        P.dma("sp", cw[k][:], w_, writes=[k + "cw"])
        P.dma("sp", cb[k][:], b_, writes=[k + "cb"])
    P.dma("sp", TRI[0][:], Uin, writes=["TRI0"])
    P.dma("sp", TRI[1][:], Lin, writes=["TRI1"])
    P.dma("pool", idb[:], idn, writes=["idb"])
    P.dma("sp", dt[:], dtr.rearrange("t p e -> p t e"), writes=["dt"])
    P.dma("sp", dtb_s[:], dtb[0:1, :].partition_broadcast(128), writes=["dtb"])
    P.dma("sp", A_s[:], alog[0:1, :].partition_broadcast(128), writes=["A_s"])
    P.dma("sp", dsk_s[:], dsk[0:1, :].partition_broadcast(128), writes=["dsk"])
    P.dma("sp", nw_s[:], nw[0:1, :].partition_broadcast(128), writes=["nw"])
    P.op("dve", lambda e: e.memset(ones[:], 1.0), writes=["ones"])
    for d in range(2):
        for j in range(2):
            P.op("pool", lambda e, d=d, j=j: e.memset(hst[d][j][:], 0.0), writes=[f"hst{d}{j}"])
            P.op("pool", lambda e, d=d, j=j: e.memset(hstb[d][j][:], 0.0), writes=[f"hstb{d}{j}"])
    P.op("dve", lambda e: e.tensor_tensor(out=dt[:], in0=dt[:], in1=dtb_s[:].unsqueeze(1).broadcast_to([128, NKT, 4]), op=ALU.add), reads=["dt", "dtb"], writes=["dt"])
    P.op("act", lambda e: e.activation(out=dt[:], in_=dt[:], func=AF.Exp), reads=["dt"], writes=["dt"])
    P.op("act", lambda e: e.activation(out=dt[:], in_=dt[:], func=AF.Ln, bias=1.0), reads=["dt"], writes=["dt"])
    P.op("act", lambda e: e.activation(out=A_s[:], in_=A_s[:], func=AF.Exp), reads=["A_s"], writes=["A_s"])
    P.op("dve", lambda e: e.scalar_tensor_tensor(out=av[:], in0=dt[:], scalar=-1.0, in1=A_s[:].unsqueeze(1).broadcast_to([128, NKT, 4]), op0=ALU.mult, op1=ALU.mult),
         reads=["dt", "A_s"], writes=["av"])
    _conv_silu(P, nc, 128, xTp, cw["x"], cb["x"], stage_in, stage_acc, xT, "x")
    _conv_silu(P, nc, 64, BTp, cw["B"], cb["B"], stage_in, stage_acc, BT, "B")
    _conv_silu(P, nc, 64, CTp, cw["C"], cb["C"], stage_in, stage_acc, CT, "C")
    for c0 in range(0, NKT, 8):
        n = min(8, NKT - c0)

        def trx(e, c0=c0, n=n):
            for i in range(n):
                ins = e.transpose(p_tr[:, i, :], xT[:, (c0 + i) * 128:(c0 + i + 1) * 128], idb[:])
            return ins
        P.op("pe", trx, reads=["xT", "idb"], writes=["p_tr"])
        P.op("act", lambda e, c0=c0, n=n: e.copy(out=x_tok[:, c0:c0 + n, :], in_=p_tr[:, 0:n, :]), reads=["p_tr"], writes=["x_tok"])

        def trb(e, c0=c0, n=n):
            for i in range(n):
                ins = e.transpose(p_tr[:, i, 0:64], BT[:, (c0 + i) * 128:(c0 + i + 1) * 128], idb[0:64, 0:64])
            return ins
        P.op("pe", trb, reads=["BT", "idb"], writes=["p_tr"])
        P.op("dve", lambda e, c0=c0, n=n: e.tensor_copy(out=B_tok[:, c0:c0 + n, :], in_=p_tr[:, 0:n, 0:64]), reads=["p_tr"], writes=["B_tok"])

    order = [(0, c) for c in range(NKT)] + [(1, c) for c in (1, 0)] + [(1, c) for c in range(NKT - 1, 1, -1)]
    for (d, c) in order:
        tri, trn = TRI[d], f"TRI{d}"
        lc = 127 if d == 0 else 0
        cs = slice(c * 128, (c + 1) * 128)
        P.op("pe", lambda e, tri=tri, c=c, d=d: e.matmul(p_col[:, 0:2], tri[:], av[:, c, 2 * d:2 * d + 2], start=True, stop=True), reads=[trn, "av"], writes=["p_col"])
        P.op("dve", lambda e: e.tensor_scalar(out=acsc[:], in0=p_col[:, 0:2], scalar1=-1.0, scalar2=None, op0=ALU.mult), reads=["p_col"], writes=["acsc"])
        P.op("pe", lambda e, c=c: e.matmul(p_cb[:, 0:128], BT[:, c * 128:(c + 1) * 128], CT[:, c * 128:(c + 1) * 128], start=True, stop=True), reads=["BT", "CT"], writes=["p_cb"])
        for j in range(2):
            col = 2 * d + j
            P.op("dve", lambda e, j=j, c=c, col=col: e.tensor_scalar(out=Abc[j][:], in0=ones[:], scalar1=av[:, c, col:col + 1], scalar2=None, op0=ALU.mult),
                 reads=["ones", "av"], writes=[f"Abc{j}"])
            P.op("pe", lambda e, j=j, tri=tri: e.matmul(p_row[j][:, 0:128], Abc[j][:], tri[:], start=True, stop=True), reads=[f"Abc{j}", trn], writes=[f"p_row{j}"])
            P.op("dve", lambda e, j=j: e.tensor_scalar(out=sg[j][:], in0=p_row[j][:, 0:128], scalar1=acsc[:, j:j + 1], scalar2=0.0, op0=ALU.add, op1=ALU.min),
                 reads=[f"p_row{j}", "acsc"], writes=[f"sg{j}"])
            P.op("act", lambda e, j=j: e.activation(out=dec[j][:], in_=sg[j][:], func=AF.Exp), reads=[f"sg{j}"], writes=[f"dec{j}"])
            P.op("pool", lambda e, j=j, tri=tri: e.tensor_tensor(out=t1[j][:], in0=dec[j][:], in1=tri[:], op=ALU.mult), reads=[f"dec{j}", trn], writes=[f"t1{j}"])
            P.op("dve", lambda e, j=j, c=c, col=col: e.scalar_tensor_tensor(out=scb[j][:], in0=p_cb[:, 0:128], scalar=dt[:, c, col:col + 1], in1=t1[j][:], op0=ALU.mult, op1=ALU.mult),
                 reads=["p_cb", "dt", f"t1{j}"], writes=[f"scbf{j}"])
            P.op("act", lambda e, j=j: e.activation(out=ea[j][:], in_=p_row[j][0:64, 0:128], func=AF.Exp), reads=[f"p_row{j}"], writes=[f"ea{j}"])
            P.op("dve", lambda e, j=j, c=c: e.tensor_tensor(out=Cp[j][:], in0=CT[:, c * 128:(c + 1) * 128], in1=ea[j][:], op=ALU.mult), reads=["CT", f"ea{j}"], writes=[f"Cp{j}"])
            P.op("act", lambda e, j=j, lc=lc: e.activation(out=wl[:, j:j + 1], in_=p_row[j][:, lc:lc + 1], func=AF.Exp, bias=acsc[:, j:j + 1], scale=1.0),
                 reads=[f"p_row{j}", "acsc"], writes=[("wl", j)])
            P.op("act", lambda e, j=j, lc=lc: e.activation(out=cd[:, j:j + 1], in_=p_row[j][0:64, lc:lc + 1], func=AF.Exp), reads=[f"p_row{j}"], writes=[("cd", j)])
            P.op("dve", lambda e, j=j, c=c, col=col: e.tensor_tensor(out=wl[:, j:j + 1], in0=wl[:, j:j + 1], in1=dt[:, c, col:col + 1], op=ALU.mult), reads=[("wl", j), "dt"], writes=[("wl", j)])
            P.op("dve", lambda e, j=j, c=c: e.tensor_scalar(out=xw[j][:], in0=x_tok[:, c, j * 64:(j + 1) * 64], scalar1=wl[:, j:j + 1], scalar2=None, op0=ALU.mult),
                 reads=["x_tok", ("wl", j)], writes=[f"xw{j}"])

        def ymm(e, c=c, d=d):
            for j in range(2):
                e.matmul(p_y[:, j * 64:(j + 1) * 64], scb[j][:], x_tok[:, c, j * 64:(j + 1) * 64], start=(j == 0), stop=False, skip_group_check=True)
                ins = e.matmul(p_y[:, j * 64:(j + 1) * 64], Cp[j][:], hstb[d][j][:], start=False, stop=True, skip_group_check=True)
            return ins
        P.op("pe", ymm, reads=["scbf0", "scbf1", "x_tok", "Cp0", "Cp1", f"hstb{d}0", f"hstb{d}1"], writes=["p_y"])
        if d == 0:
            P.op("pool", lambda e, c=c: e.tensor_tensor(out=tmpy[:], in0=x_tok[:, c, :], in1=dsk_s[:], op=ALU.mult), reads=["x_tok", "dsk"], writes=["tmpy"])
            P.op("dve", lambda e, c=c: e.tensor_tensor(out=yacc[:, c, :], in0=p_y[:, 0:128], in1=tmpy[:], op=ALU.add), reads=["p_y", "tmpy"], writes=[("yacc", c)])
        else:
            P.op("dve", lambda e, c=c: e.tensor_tensor(out=yacc[:, c, :], in0=p_y[:, 0:128], in1=yacc[:, c, :], op=ALU.add), reads=["p_y", ("yacc", c)], writes=[("yacc", c)])

        def smm(e, c=c):
            for j in range(2):
                ins = e.matmul(p_st[0:64, j * 64:(j + 1) * 64], B_tok[:, c, :], xw[j][:], start=(j == 0), stop=(j == 1), skip_group_check=True)
            return ins
        P.op("pe", smm, reads=["B_tok", "xw0", "xw1"], writes=["p_st"])
        for j in range(2):
            P.op("dve", lambda e, j=j, d=d: e.scalar_tensor_tensor(out=hst[d][j][:], in0=hst[d][j][:], scalar=cd[:, j:j + 1], in1=p_st[0:64, j * 64:(j + 1) * 64], op0=ALU.mult, op1=ALU.add),
                 reads=[f"hst{d}{j}", ("cd", j), "p_st"], writes=[f"hst{d}{j}"])
            P.op("act", lambda e, j=j, d=d: e.copy(out=hstb[d][j][:], in_=hst[d][j][:]), reads=[f"hst{d}{j}"], writes=[f"hstb{d}{j}"])

    yall = [("yacc", c) for c in range(NKT)]
    zt = stage_in.ap() if hasattr(stage_in, "ap") else stage_in
    zv = stage_in[:, 0:NKT * 128].rearrange("p (t e) -> p t e", e=128)
    sqv = stage_acc[:, 0:NKT * 128].rearrange("p (t e) -> p t e", e=128)
    P.dma("sp", zv, zin.rearrange("t p e -> p t e"), writes=["stage_in"])
    P.op("act", lambda e: e.activation(out=zv, in_=zv, func=AF.Silu), reads=["stage_in"], writes=["stage_in"])
    P.op("dve", lambda e: e.tensor_tensor(out=yacc[:], in0=yacc[:], in1=zv, op=ALU.mult), reads=yall + ["stage_in"], writes=yall)
    P.op("pool", lambda e: e.tensor_tensor(out=sqv, in0=yacc[:], in1=yacc[:], op=ALU.mult), reads=yall, writes=["stage_acc"])
    P.op("dve", lambda e: e.tensor_reduce(out=rs_[:], in_=sqv, axis=AX.X, op=ALU.add), reads=["stage_acc"], writes=["rsS"])
    P.op("dve", lambda e: e.tensor_scalar(out=rs_[:], in0=rs_[:], scalar1=1.0 / 128, scalar2=EPS, op0=ALU.mult, op1=ALU.add), reads=["rsS"], writes=["rsS"])
    P.op("act", lambda e: e.activation(out=rs_[:], in_=rs_[:], func=AF.Sqrt), reads=["rsS"], writes=["rsS"])
    P.op("dve", lambda e: e.reciprocal(out=rs_[:], in_=rs_[:]), reads=["rsS"], writes=["rsS"])
    P.op("dve", lambda e: e.tensor_tensor(out=yacc[:], in0=yacc[:], in1=rs_[:].unsqueeze(2).broadcast_to([128, NKT, 128]), op=ALU.mult), reads=yall + ["rsS"], writes=yall)
    P.op("pool", lambda e: e.tensor_tensor(out=yacc[:], in0=yacc[:], in1=nw_s[:].unsqueeze(1).broadcast_to([128, NKT, 128]), op=ALU.mult), reads=yall + ["nw"], writes=yall)
    P.dma("sp", sout.rearrange("t p e -> p t e"), yacc[:], reads=yall, writes=["sout"])
    P.finish()
    P.emit()
    return nc


def build_MG():
    nc = bass.Bass("TRN2", target_bir_lowering=False)
    qTd = _din(nc, "qT", [32, TT])
    kTd = _din(nc, "kT", [32, TT])
    ktd = _din(nc, "ktok", [NKT, 128, 32])
    vtd = _din(nc, "vtok", [NKT, 128, 64])
    gtd = _din(nc, "gtok", [NKT, 128, 64])
    cTd = [_din(nc, f"codeT{d}", [16, TT]) for d in range(2)]
    upd = [_din(nc, f"up{d}", [16, 32]) for d in range(2)]
    gbd = [_din(nc, f"gkb{d}", [1, 32]) for d in range(2)]
    nw = _din(nc, "nw", [1, 64])
    Uin = _din(nc, "U", [128, 128])
    Lin = _din(nc, "L", [128, 128])
    cout = _dout(nc, "cout", [NKT, 128, 64])
    P = Prog(nc)
    A = nc.alloc_sbuf_tensor
    PS = nc.alloc_psum_tensor
    qT = A("qTs", [32, TT], F32)
    kT = A("kTs", [32, TT], F32)
    ktok = A("ktoks", [128, NKT, 32], F32)
    vtok = A("vtoks", [128, NKT, 64], BF16)
    gtok = A("gtoks", [128, NKT, 64], F32)
    cT = [A(f"cTs{d}", [16, TT], F32) for d in range(2)]
    up = [A(f"ups{d}", [16, 32], F32) for d in range(2)]
    gb = [A(f"gbs{d}", [1, 32], F32) for d in range(2)]
    nw_s = A("nwG", [128, 64], F32)
    TRI = [A("U_g", [128, 128], F32), A("L_g", [128, 128], F32)]
    SL = [A("SL0", [128, 128], F32), A("SL1", [128, 128], F32)]
    ones = A("onesG", [1, 128], F32)
    gk = A("gk", [128, NKT, 64], F32)
    eb = A("eb", [32, 128], F32)
    enb = A("enb", [32, 128], F32)
    qt = A("qt", [32, 128], BF16)
    kt = A("kt", [32, 128], BF16)
    attm = A("attm", [128, 128], BF16)
    eblb = A("eblb", [128, 32], F32)
    kend = A("kend", [128, 32], BF16)
    cdec = A("cdec", [32, 1], F32)
    S = [A(f"S{d}", [32, 64], F32) for d in range(2)]
    Sb = [A(f"Sb{d}", [32, 64], BF16) for d in range(2)]
    oacc = A("oacc", [128, NKT, 64], F32)
    rs_ = A("rsG", [128, NKT], F32)
    p_pre = PS("p_pre", [128, 512], F32)
    p_bT = PS("p_bT", [128, 512], F32)
    p_blb = PS("p_blb", [128, 512], F32)
    p_att = PS("p_att", [128, 512], F32)
    p_o = PS("p_o", [128, 512], F32)
    p_s = PS("p_s", [128, 512], F32)
    scale = 32 ** -0.5

    P.dma("sp", qT[:], qTd, writes=["qT"])
    P.dma("sp", kT[:], kTd, writes=["kT"])
    P.dma("sp", ktok[:], ktd.rearrange("t p e -> p t e"), writes=["ktok"])
    P.dma("pool", vtok[:], vtd.rearrange("t p e -> p t e"), writes=["vtok"])
    P.dma("sp", gtok[:], gtd.rearrange("t p e -> p t e"), writes=["gtok"])
    for d in range(2):
        P.dma("sp", cT[d][:], cTd[d], writes=[f"cT{d}"])
        P.dma("sp", up[d][:], upd[d], writes=[f"up{d}"])
        P.dma("sp", gb[d][:], gbd[d], writes=[f"gb{d}"])
    P.dma("sp", nw_s[:], nw[0:1, :].partition_broadcast(128), writes=["nw"])
    P.dma("sp", TRI[0][:], Uin, writes=["TRI0"])
    P.dma("sp", TRI[1][:], Lin, writes=["TRI1"])
    P.op("dve", lambda e: e.memset(ones[:], 1.0), writes=["ones"])
    for d in range(2):
        P.op("dve", lambda e, d=d: e.tensor_scalar(out=SL[d][:], in0=TRI[d][:], scalar1=-1.0, scalar2=1.0, op0=ALU.mult, op1=ALU.add), reads=[f"TRI{d}"], writes=[f"SL{d}"])
        P.op("pool", lambda e, d=d: e.memset(S[d][:], 0.0), writes=[f"S{d}"])
        P.op("pool", lambda e, d=d: e.memset(Sb[d][:], 0.0), writes=[f"Sb{d}"])
    for c in range(NKT):
        def pre(e, c=c):
            for d in range(2):
                e.matmul(p_pre[:, d * 32:(d + 1) * 32], cT[d][:, c * 128:(c + 1) * 128], up[d][:], start=(d == 0), stop=False, skip_group_check=True)
                ins = e.matmul(p_pre[:, d * 32:(d + 1) * 32], ones[0:1, :], gb[d][:], start=False, stop=True, skip_group_check=True)
            return ins
        P.op("pe", pre, reads=["cT0", "cT1", "up0", "up1", "gb0", "gb1", "ones"], writes=["p_pre"])
        P.op("act", lambda e, c=c: e.activation(out=gk[:, c, :], in_=p_pre[:, 0:64], func=AF.Exp, scale=-1.0), reads=["p_pre"], writes=[("gk", c)])
    gkall = [("gk", c) for c in range(NKT)]
    P.op("act", lambda e: e.activation(out=gk[:], in_=gk[:], func=AF.Ln, bias=1.0), reads=gkall, writes=gkall)
    P.op("dve", lambda e: e.tensor_scalar(out=gk[:], in0=gk[:], scalar1=-1.0 / 16.0, scalar2=None, op0=ALU.mult), reads=gkall, writes=gkall)

    order = [(0, c) for c in range(NKT)] + [(1, c) for c in (1, 0)] + [(1, c) for c in range(NKT - 1, 1, -1)]
    for (d, c) in order:
        tri, trn = TRI[d], f"TRI{d}"
        lc = 127 if d == 0 else 0
        cs = slice(c * 128, (c + 1) * 128)
        gkc = gk[:, c, d * 32:(d + 1) * 32]
        P.op("pe", lambda e, gkc=gkc, tri=tri: e.matmul(p_bT[0:32, 0:128], gkc, tri[:], start=True, stop=True), reads=[("gk", c), trn], writes=["p_bT"])
        P.op("pe", lambda e, gkc=gkc, d=d: e.matmul(p_blb[:, 0:32], SL[d][:], gkc, start=True, stop=True), reads=[("gk", c), f"SL{d}"], writes=["p_blb"])
        P.op("act", lambda e: e.activation(out=eb[:], in_=p_bT[0:32, 0:128], func=AF.Exp), reads=["p_bT"], writes=["eb"])
        P.op("act", lambda e: e.activation(out=enb[:], in_=p_bT[0:32, 0:128], func=AF.Exp, scale=-1.0), reads=["p_bT"], writes=["enb"])
        P.op("act", lambda e, lc=lc: e.activation(out=cdec[:], in_=p_bT[0:32, lc:lc + 1], func=AF.Exp), reads=["p_bT"], writes=["cdec"])
        P.op("act", lambda e: e.activation(out=eblb[:], in_=p_blb[:, 0:32], func=AF.Exp), reads=["p_blb"], writes=["eblb"])
        P.op("dve", lambda e, cs=cs: e.scalar_tensor_tensor(out=qt[:], in0=qT[:, cs], scalar=scale, in1=eb[:], op0=ALU.mult, op1=ALU.mult), reads=["qT", "eb"], writes=["qt"])
        P.op("dve", lambda e, cs=cs: e.tensor_tensor(out=kt[:], in0=kT[:, cs], in1=enb[:], op=ALU.mult), reads=["kT", "enb"], writes=["kt"])
        P.op("pe", lambda e: e.matmul(p_att[:, 0:128], kt[:], qt[:], start=True, stop=True), reads=["kt", "qt"], writes=["p_att"])
        P.op("dve", lambda e, tri=tri: e.tensor_tensor(out=attm[:], in0=p_att[:, 0:128], in1=tri[:], op=ALU.mult), reads=["p_att", trn], writes=["attm"])

        def omm(e, c=c, d=d):
            e.matmul(p_o[:, 0:64], attm[:], vtok[:, c, :], start=True, stop=False)
            return e.matmul(p_o[:, 0:64], qt[:], Sb[d][:], start=False, stop=True)
        P.op("pe", omm, reads=["attm", "vtok", "qt", f"Sb{d}"], writes=["p_o"])
        if d == 0:
            P.op("act", lambda e, c=c: e.copy(out=oacc[:, c, :], in_=p_o[:, 0:64]), reads=["p_o"], writes=[("oacc", c)])
        else:
            P.op("dve", lambda e, c=c: e.tensor_tensor(out=oacc[:, c, :], in0=p_o[:, 0:64], in1=oacc[:, c, :], op=ALU.add), reads=["p_o", ("oacc", c)], writes=[("oacc", c)])
        P.op("pool", lambda e, c=c: e.tensor_tensor(out=kend[:], in0=ktok[:, c, :], in1=eblb[:], op=ALU.mult), reads=["ktok", "eblb"], writes=["kend"])
        P.op("pe", lambda e, c=c: e.matmul(p_s[0:32, 0:64], kend[:], vtok[:, c, :], start=True, stop=True), reads=["kend", "vtok"], writes=["p_s"])
        P.op("dve", lambda e, d=d: e.scalar_tensor_tensor(out=S[d][:], in0=S[d][:], scalar=cdec[:, 0:1], in1=p_s[0:32, 0:64], op0=ALU.mult, op1=ALU.add),
             reads=[f"S{d}", "cdec", "p_s"], writes=[f"S{d}"])
        P.op("act", lambda e, d=d: e.copy(out=Sb[d][:], in_=S[d][:]), reads=[f"S{d}"], writes=[f"Sb{d}"])

    oall = [("oacc", c) for c in range(NKT)]
    P.op("pool", lambda e: e.tensor_tensor(out=gk[:], in0=oacc[:], in1=oacc[:], op=ALU.mult), reads=oall, writes=gkall)
    P.op("dve", lambda e: e.tensor_reduce(out=rs_[:], in_=gk[:], axis=AX.X, op=ALU.add), reads=gkall, writes=["rsG"])
    P.op("dve", lambda e: e.tensor_scalar(out=rs_[:], in0=rs_[:], scalar1=1.0 / 64, scalar2=EPS, op0=ALU.mult, op1=ALU.add), reads=["rsG"], writes=["rsG"])
    P.op("act", lambda e: e.activation(out=rs_[:], in_=rs_[:], func=AF.Sqrt), reads=["rsG"], writes=["rsG"])
    P.op("dve", lambda e: e.reciprocal(out=rs_[:], in_=rs_[:]), reads=["rsG"], writes=["rsG"])
    P.op("act", lambda e: e.activation(out=gtok[:], in_=gtok[:], func=AF.Silu), reads=["gtok"], writes=["gtok"])
    P.op("dve", lambda e: e.tensor_tensor(out=oacc[:], in0=oacc[:], in1=rs_[:].unsqueeze(2).broadcast_to([128, NKT, 64]), op=ALU.mult), reads=oall + ["rsG"], writes=oall)
    P.op("pool", lambda e: e.tensor_tensor(out=oacc[:], in0=oacc[:], in1=nw_s[:].unsqueeze(1).broadcast_to([128, NKT, 64]), op=ALU.mult), reads=oall + ["nw"], writes=oall)
    P.op("dve", lambda e: e.tensor_tensor(out=oacc[:], in0=oacc[:], in1=gtok[:], op=ALU.mult), reads=oall + ["gtok"], writes=oall)
    P.dma("sp", cout.rearrange("t p e -> p t e"), oacc[:], reads=oall, writes=["cout"])
    P.finish()
    P.emit()
    return nc


def build_O():
    nc = bass.Bass("TRN2", target_bir_lowering=False)
    xs = _din(nc, "xs", [NTI, 128, D])
    mix = _din(nc, "mix", [NTI, 128, D])
    mol = _din(nc, "mol", [4, D])
    moc = _din(nc, "moc", [4, D])
    n2w = _din(nc, "n2w", [1, D])
    w_out = _din(nc, "w_out", [D, D])
    wr = _din(nc, "wr", [D, 32])
    br = _din(nc, "br", [1, 32])
    idn = _din(nc, "idn", [128, 128])
    xmid = _dout(nc, "xmid", [NTI, 128, D])
    h2T = _dout(nc, "h2T", [128, 8, NTI * 128], F32)
    gates = _dout(nc, "gates", [NTI, 128, 32])
    P = Prog(nc)
    A = nc.alloc_sbuf_tensor
    PS = nc.alloc_psum_tensor
    W = A("Wo", [128, 8, D], BF16)
    wrs = A("wrs", [128, 8, 32], F32)
    brs = A("brs", [1, 32], F32)
    ones = A("onesO", [1, 128], F32)
    idb = A("idbO", [128, 128], BF16)
    idf = A("idfO", [128, 128], F32)
    nwb = A("nwbO", [128, D], F32)
    scb = A("scbO", [128, D], F32)
    G1 = [A(f"G1{j}", [128, D], F32) for j in range(2)]
    G2 = [A(f"G2{j}", [128, D], F32) for j in range(2)]
    SH2 = [A(f"SH2{j}", [128, D], F32) for j in range(2)]
    xts = [A(f"xtO{j}", [128, D], F32) for j in range(2)]
    mxb = [A(f"mxb{j}", [128, D], BF16) for j in range(2)]
    mT = A("mT", [128, 8, 128], BF16)
    tmp = A("tmpO", [128, D], F32)
    xm = [A(f"xm{j}", [128, D], F32) for j in range(2)]
    junk = A("junkO", [128, D], F32)
    h2 = A("h2", [128, D], F32)
    h2T32s = [A(f"h2T32_{j}", [128, 8, 128], F32) for j in range(2)]
    ss = A("ssO", [128, NTI], F32)
    rstd = A("rstdO", [128, NTI], F32)
    lg = A("lg", [128, 32], F32)
    t8 = A("t8", [128, 8], F32)
    nmx = A("nmx", [128, 1], F32)
    msk = A("msk", [128, 32], F32)
    ex = A("ex", [128, 32], F32)
    sm = A("sm", [128, 1], F32)
    gt = [A(f"gt{j}", [128, 32], F32) for j in range(2)]
    pst = PS("pstO", [128, 8, 128], BF16)
    pso = [PS(f"psoO{j}", [128, 512], F32) for j in range(2)]
    pt32 = [PS(f"pt32{j}", [128, 4, 128], F32) for j in range(2)]
    plg = PS("plg", [128, 512], F32)

    for k in range(8):
        P.dma("pool", W[:, k, :], w_out[k * 128:(k + 1) * 128, :], writes=[("W", k)])
    P.dma("sp", wrs[:], wr.rearrange("(k p) n -> p k n", p=128), writes=["wrs"])
    P.dma("sp", brs[:], br, writes=["brs"])
    P.dma("pool", idb[:], idn, writes=["idb"])
    P.dma("sp", idf[:], idn, writes=["idf"])
    P.dma("sp", nwb[:], n2w[0:1, :].partition_broadcast(128), writes=["nwb"])
    P.op("dve", lambda e: e.memset(ones[:], 1.0), writes=["ones"])
    for j, m in enumerate((mol, moc)):
        P.dma("sp", G1[j][:], m[0:1, :].partition_broadcast(128), writes=[f"G1{j}"])
        P.dma("sp", SH2[j][:], m[1:2, :].partition_broadcast(128), writes=[f"SH2{j}"])
        P.dma("sp", scb[:], m[2:3, :].partition_broadcast(128), writes=["scb"])
        P.op("dve", lambda e, j=j: e.scalar_tensor_tensor(out=G2[j][:], in0=scb[:], scalar=1.0, in1=nwb[:], op0=ALU.add, op1=ALU.mult),
             reads=["scb", "nwb"], writes=[f"G2{j}"])
    Wall = [("W", k) for k in range(8)]
    for i in range(NTI):
        j = 0 if i < 16 else 1
        xt, xn = xts[i % 2], f"xtO{i % 2}"
        mb, mn = mxb[i % 2], f"mxb{i % 2}"
        xmt, xmn = xm[i % 2], f"xm{i % 2}"
        P.dma("sp", xt[:], xs[i], writes=[xn])
        P.dma("pool", mb[:], mix[i], writes=[mn])

        def tr(e, mb=mb):
            for k in range(8):
                ins = e.transpose(pst[:, k, :], mb[:, k * 128:(k + 1) * 128], idb[:])
            return ins
        P.op("pe", tr, reads=[mn, "idb"], writes=["pst"])
        P.op("act", lambda e: e.copy(out=mT[:], in_=pst[:]), reads=["pst"], writes=["mT"])
        for hh in range(2):
            ps, pn = pso[hh], f"psoO{hh}"

            def mm(e, ps=ps, hh=hh):
                for k in range(8):
                    ins = e.matmul(ps[:], mT[:, k, :], W[:, k, hh * 512:(hh + 1) * 512], start=(k == 0), stop=(k == 7))
                return ins
            P.op("pe", mm, reads=["mT"] + Wall, writes=[pn])
            P.op("dve", lambda e, ps=ps, hh=hh, j=j: e.tensor_tensor(out=tmp[:, hh * 512:(hh + 1) * 512], in0=ps[:], in1=G1[j][:, hh * 512:(hh + 1) * 512], op=ALU.mult),
                 reads=[pn, f"G1{j}"], writes=[("tmpO", hh)])
        P.op("pool", lambda e, xt=xt, xmt=xmt: e.tensor_tensor(out=xmt[:], in0=xt[:], in1=tmp[:], op=ALU.add), reads=[xn, ("tmpO", 0), ("tmpO", 1)], writes=[xmn])
        P.dma("sp", xmid[i], xmt[:], reads=[xmn], writes=[("xmid", i)])
        P.op("act", lambda e, xmt=xmt, i=i: e.activation(out=junk[:], in_=xmt[:], func=AF.Square, accum_out=ss[:, i:i + 1]), reads=[xmn], writes=["junkO", ("ss", i)])
        P.op("dve", lambda e, i=i: e.tensor_scalar(out=rstd[:, i:i + 1], in0=ss[:, i:i + 1], scalar1=1.0 / D, scalar2=EPS, op0=ALU.mult, op1=ALU.add), reads=[("ss", i)], writes=[("rstd", i)])
        P.op("act", lambda e, i=i: e.activation(out=rstd[:, i:i + 1], in_=rstd[:, i:i + 1], func=AF.Sqrt), reads=[("rstd", i)], writes=[("rstd", i)])
        P.op("dve", lambda e, i=i: e.reciprocal(out=rstd[:, i:i + 1], in_=rstd[:, i:i + 1]), reads=[("rstd", i)], writes=[("rstd", i)])
        P.op("dve", lambda e, xmt=xmt, i=i, j=j: e.scalar_tensor_tensor(out=h2[:], in0=xmt[:], scalar=rstd[:, i:i + 1], in1=G2[j][:], op0=ALU.mult, op1=ALU.mult),
             reads=[xmn, ("rstd", i), f"G2{j}"], writes=["h2"])
        P.op("pool", lambda e, j=j: e.tensor_tensor(out=h2[:], in0=h2[:], in1=SH2[j][:], op=ALU.add), reads=["h2", f"SH2{j}"], writes=["h2"])
        h2T32, hbn = h2T32s[i % 2], f"h2T32_{i % 2}"
        for hh in range(2):
            pt, ptn = pt32[hh], f"pt32{hh}"

            def tr32(e, pt=pt, hh=hh):
                for k in range(4):
                    ins = e.transpose(pt[:, k, :], h2[:, (hh * 4 + k) * 128:(hh * 4 + k + 1) * 128], idf[:])
                return ins
            P.op("pe", tr32, reads=["h2", "idf"], writes=[ptn])
            P.op("act", lambda e, pt=pt, hh=hh, h2T32=h2T32: e.copy(out=h2T32[:, hh * 4:(hh + 1) * 4, :], in_=pt[:]), reads=[ptn], writes=[(hbn, hh)])
        P.dma("sp", h2T[:, :, i * 128:(i + 1) * 128], h2T32[:], reads=[(hbn, 0), (hbn, 1)], writes=[("h2T", i)])

        def rmm(e, h2T32=h2T32):
            for k in range(8):
                e.matmul(plg[:, 0:32], h2T32[:, k, :], wrs[:, k, :], start=(k == 0), stop=False)
            return e.matmul(plg[:, 0:32], ones[0:1, :], brs[:], start=False, stop=True)
        P.op("pe", rmm, reads=[(hbn, 0), (hbn, 1), "wrs", "brs", "ones"], writes=["plg"])
        g, gn = gt[i % 2], f"gt{i % 2}"
        P.op("act", lambda e: e.copy(out=lg[:], in_=plg[:, 0:32]), reads=["plg"], writes=["lg"])
        P.op("dve", lambda e: e.max(out=t8[:], in_=lg[:]), reads=["lg"], writes=["t8"])
        P.op("dve", lambda e: e.tensor_scalar(out=nmx[:], in0=t8[:, 0:1], scalar1=-1.0, scalar2=None, op0=ALU.mult), reads=["t8"], writes=["nmx"])
        P.op("dve", lambda e: e.tensor_scalar(out=msk[:], in0=lg[:], scalar1=t8[:, 3:4], scalar2=None, op0=ALU.is_ge), reads=["lg", "t8"], writes=["msk"])
        P.op("act", lambda e: e.activation(out=ex[:], in_=lg[:], func=AF.Exp, bias=nmx[:, 0:1], scale=1.0), reads=["lg", "nmx"], writes=["ex"])
        P.op("dve", lambda e: e.tensor_tensor(out=ex[:], in0=ex[:], in1=msk[:], op=ALU.mult), reads=["ex", "msk"], writes=["ex"])
        P.op("dve", lambda e: e.tensor_reduce(out=sm[:], in_=ex[:], axis=AX.X, op=ALU.add), reads=["ex"], writes=["sm"])
        P.op("dve", lambda e: e.reciprocal(out=sm[:], in_=sm[:]), reads=["sm"], writes=["sm"])
        P.op("dve", lambda e, g=g: e.tensor_scalar(out=g[:], in0=ex[:], scalar1=sm[:, 0:1], scalar2=None, op0=ALU.mult), reads=["ex", "sm"], writes=[gn])
        P.dma("sp", gates[i], g[:], reads=[gn], writes=[("gates", i)])
    P.finish()
    P.emit()
    return nc


NTOK = NCORES * NTI * 128
NBLK = NTOK // 512
NEL = 4


def build_E():
    nc = bass.Bass("TRN2", target_bir_lowering=False)
    hT = _din(nc, "hT", [128, 8, NTOK], F32)
    gate = _din(nc, "gate", [NTOK // 128, 128, NEL])
    wgu = _din(nc, "wgu", [NEL, D, 2048])
    bguT = _din(nc, "bguT", [NEL, 128, 16])
    wd = _din(nc, "wd", [NEL, D, D])
    bd = _din(nc, "bd", [NEL, D])
    ffp = _dout(nc, "ffp", [NTOK // 128, 128, D])
    P = Prog(nc)
    A = nc.alloc_sbuf_tensor
    PS = nc.alloc_psum_tensor
    Wgu = [A(f"Wgu{j}", [128, 8, 2048], BF16) for j in range(2)]
    Wd = [A(f"Wd{j}", [128, 8, D], BF16) for j in range(2)]
    bgs = [A(f"bgs{j}", [128, 16], F32) for j in range(2)]
    bds = [A(f"bds{j}", [128, D], F32) for j in range(2)]
    gts = A("gts", [128, NTOK // 128, NEL], F32)
    hTs = [A(f"hTs{j}", [128, 8, 512], BF16) for j in range(2)]
    actT = A("actT", [128, 8, 512], BF16)
    glu = [A(f"glu{j}", [128, 512], F32) for j in range(2)]
    sig = [A(f"sig{j}", [128, 512], F32) for j in range(2)]
    lin = [A(f"lin{j}", [128, 512], F32) for j in range(2)]
    tmpd = [A(f"tmpd{j}", [128, 512], F32) for j in range(2)]
    facc = [A(f"facc{j}", [128, D], F32) for j in range(2)]
    psg = [PS(f"psg{j}", [128, 512], F32) for j in range(2)]
    psl = [PS(f"psl{j}", [128, 512], F32) for j in range(2)]
    psd = [PS(f"psd{j}", [128, 512], F32) for j in range(2)]
    P.dma("sp", gts[:], gate.rearrange("t p e -> p t e"), writes=["gts"])

    def load_w(e_):
        j = e_ % 2
        for k in range(8):
            P.dma("pool", Wgu[j][:, k, :], wgu[e_, k * 128:(k + 1) * 128, :], writes=[(f"Wgu{j}", k)])
            P.dma("pool", Wd[j][:, k, :], wd[e_, k * 128:(k + 1) * 128, :], writes=[(f"Wd{j}", k)])
        P.dma("sp", bgs[j][:], bguT[e_], writes=[f"bgs{j}"])
        P.dma("sp", bds[j][:], bd[e_:e_ + 1, :].partition_broadcast(128), writes=[f"bds{j}"])

    load_w(0)
    fi = 0
    for e_ in range(NEL):
        j = e_ % 2
        if e_ + 1 < NEL:
            load_w(e_ + 1)
        WguA = [(f"Wgu{j}", k) for k in range(8)]
        WdA = [(f"Wd{j}", k) for k in range(8)]
        for blk in range(NBLK):
            hb, hbn = hTs[blk % 2], f"hTs{blk % 2}"
            P.dma("pool", hb[:], hT[:, :, blk * 512:(blk + 1) * 512], writes=[hbn])
            for fc in range(8):
                b2 = fc % 2
                pg, pgn = psg[b2], f"psg{b2}"
                pl, pln = psl[b2], f"psl{b2}"

                def mg(e, pg=pg, fc=fc, hb=hb, j=j):
                    for k in range(8):
                        ins = e.matmul(pg[:], Wgu[j][:, k, fc * 128:(fc + 1) * 128], hb[:, k, :], start=(k == 0), stop=(k == 7))
                    return ins

                def ml_(e, pl=pl, fc=fc, hb=hb, j=j):
                    for k in range(8):
                        ins = e.matmul(pl[:], Wgu[j][:, k, 1024 + fc * 128:1024 + (fc + 1) * 128], hb[:, k, :], start=(k == 0), stop=(k == 7))
                    return ins
                P.op("pe", mg, reads=[hbn] + WguA, writes=[pgn])
                P.op("pe", ml_, reads=[hbn] + WguA, writes=[pln])
                P.op("dve", lambda e, pg=pg, fc=fc, b2=b2, j=j: e.tensor_scalar(out=glu[b2][:], in0=pg[:], scalar1=bgs[j][:, fc:fc + 1], scalar2=7.0, op0=ALU.add, op1=ALU.min),
                     reads=[pgn, f"bgs{j}"], writes=[f"glu{b2}"])
                P.op("act", lambda e, b2=b2: e.activation(out=sig[b2][:], in_=glu[b2][:], func=AF.Sigmoid, scale=1.702), reads=[f"glu{b2}"], writes=[f"sig{b2}"])
                P.op("dve", lambda e, pl=pl, fc=fc, b2=b2, j=j: e.tensor_scalar(out=lin[b2][:], in0=pl[:], scalar1=bgs[j][:, 8 + fc:9 + fc], scalar2=7.0, op0=ALU.add, op1=ALU.min),
                     reads=[pln, f"bgs{j}"], writes=[f"lin{b2}"])
                P.op("pool", lambda e, b2=b2: e.tensor_scalar(out=lin[b2][:], in0=lin[b2][:], scalar1=-7.0, scalar2=1.0, op0=ALU.max, op1=ALU.add), reads=[f"lin{b2}"], writes=[f"lin{b2}"])
                P.op("pool", lambda e, b2=b2: e.tensor_tensor(out=glu[b2][:], in0=glu[b2][:], in1=sig[b2][:], op=ALU.mult), reads=[f"glu{b2}", f"sig{b2}"], writes=[f"glu{b2}"])
                P.op("dve", lambda e, b2=b2, fc=fc: e.tensor_tensor(out=actT[:, fc, :], in0=glu[b2][:], in1=lin[b2][:], op=ALU.mult), reads=[f"glu{b2}", f"lin{b2}"], writes=[("actT", fc)])
            actA = [("actT", fc) for fc in range(8)]
            for tt in range(4):
                tile_i = blk * 4 + tt
                fa, fan = facc[fi % 2], f"facc{fi % 2}"
                fi += 1
                if e_ > 0:
                    P.dma("sp", fa[:], ffp[tile_i], reads=[("ffp", tile_i)], writes=[fan])
                for dh in range(2):
                    pd, pdn = psd[dh], f"psd{dh}"

                    def md(e, pd=pd, tt=tt, dh=dh, j=j):
                        for fc in range(8):
                            ins = e.matmul(pd[:], actT[:, fc, tt * 128:(tt + 1) * 128], Wd[j][:, fc, dh * 512:(dh + 1) * 512], start=(fc == 0), stop=(fc == 7))
                        return ins
                    P.op("pe", md, reads=actA + WdA, writes=[pdn])
                    P.op("dve", lambda e, pd=pd, dh=dh, j=j: e.tensor_tensor(out=tmpd[dh][:], in0=pd[:], in1=bds[j][:, dh * 512:(dh + 1) * 512], op=ALU.add),
                         reads=[pdn, f"bds{j}"], writes=[f"tmpd{dh}"])
                    if e_ == 0:
                        P.op("dve", lambda e, dh=dh, fa=fa, tile_i=tile_i, e_=e_: e.tensor_scalar(out=fa[:, dh * 512:(dh + 1) * 512], in0=tmpd[dh][:], scalar1=gts[:, tile_i, e_:e_ + 1], scalar2=None, op0=ALU.mult),
                             reads=[f"tmpd{dh}", "gts"], writes=[(fan, dh)])
                    else:
                        P.op("dve", lambda e, dh=dh, fa=fa, tile_i=tile_i, e_=e_: e.scalar_tensor_tensor(out=fa[:, dh * 512:(dh + 1) * 512], in0=tmpd[dh][:], scalar=gts[:, tile_i, e_:e_ + 1], in1=fa[:, dh * 512:(dh + 1) * 512], op0=ALU.mult, op1=ALU.add),
                             reads=[f"tmpd{dh}", "gts", fan], writes=[(fan, dh)])
                P.dma("sp", ffp[tile_i], fa[:], reads=[(fan, 0), (fan, 1), fan], writes=[("ffp", tile_i)])
    P.finish()
    P.emit()
    return nc


def build_R(final):
    nc = bass.Bass("TRN2", target_bir_lowering=False)
    xmid = _din(nc, "xmid", [NTI, 128, D])
    parts = _din(nc, "parts", [NCORES, NTI, 128, D])
    g2l = _din(nc, "g2l", [1, D])
    g2c = _din(nc, "g2c", [1, D])
    fnw = _din(nc, "fnw", [1, D])
    xnew = _dout(nc, "xnew", [NTI, 128, D])
    P = Prog(nc)
    A = nc.alloc_sbuf_tensor
    G2 = [A(f"G2R{j}", [128, D], F32) for j in range(2)]
    fw = A("fwR", [128, D], F32)
    xt = [A(f"xtR{j}", [128, D], F32) for j in range(2)]
    pt = [A(f"ptR{j}", [128, D], F32) for j in range(4)]
    acc = [A(f"accR{j}", [128, D], F32) for j in range(2)]
    junk = A("junkR", [128, D], F32)
    ss = A("ssR", [128, NTI], F32)
    P.dma("sp", G2[0][:], g2l[0:1, :].partition_broadcast(128), writes=["G2R0"])
    P.dma("sp", G2[1][:], g2c[0:1, :].partition_broadcast(128), writes=["G2R1"])
    P.dma("sp", fw[:], fnw[0:1, :].partition_broadcast(128), writes=["fwR"])
    pi = 0
    for i in range(NTI):
        j = 0 if i < 16 else 1
        x_, xn = xt[i % 2], f"xtR{i % 2}"
        a_, an = acc[i % 2], f"accR{i % 2}"
        P.dma("sp", x_[:], xmid[i], writes=[xn])
        for c in range(NCORES):
            p_, pn = pt[pi % 4], f"ptR{pi % 4}"
            pi += 1
            P.dma("sp" if c % 2 == 0 else "pool", p_[:], parts[c, i], writes=[pn])
            eng = "dve" if c % 2 == 0 else "pool"
            if c == 0:
                P.op(eng, lambda e, a_=a_, p_=p_: e.tensor_copy(out=a_[:], in_=p_[:]), reads=[pn], writes=[an])
            else:
                P.op(eng, lambda e, a_=a_, p_=p_: e.tensor_tensor(out=a_[:], in0=a_[:], in1=p_[:], op=ALU.add), reads=[pn, an], writes=[an])
        P.op("dve", lambda e, a_=a_, j=j: e.tensor_tensor(out=a_[:], in0=a_[:], in1=G2[j][:], op=ALU.mult), reads=[an, f"G2R{j}"], writes=[an])
        P.op("pool", lambda e, a_=a_, x_=x_: e.tensor_tensor(out=a_[:], in0=a_[:], in1=x_[:], op=ALU.add), reads=[an, xn], writes=[an])
        if final:
            P.op("act", lambda e, a_=a_, i=i: e.activation(out=junk[:], in_=a_[:], func=AF.Square, accum_out=ss[:, i:i + 1]), reads=[an], writes=["junkR", ("ss", i)])
            P.op("dve", lambda e, i=i: e.tensor_scalar(out=ss[:, i:i + 1], in0=ss[:, i:i + 1], scalar1=1.0 / D, scalar2=EPS, op0=ALU.mult, op1=ALU.add), reads=[("ss", i)], writes=[("ss", i)])
            P.op("act", lambda e, i=i: e.activation(out=ss[:, i:i + 1], in_=ss[:, i:i + 1], func=AF.Sqrt), reads=[("ss", i)], writes=[("ss", i)])
            P.op("dve", lambda e, i=i: e.reciprocal(out=ss[:, i:i + 1], in_=ss[:, i:i + 1]), reads=[("ss", i)], writes=[("ss", i)])
            P.op("dve", lambda e, a_=a_, i=i: e.scalar_tensor_tensor(out=a_[:], in0=a_[:], scalar=ss[:, i:i + 1], in1=fw[:], op0=ALU.mult, op1=ALU.mult), reads=[an, ("ss", i), "fwR"], writes=[an])
        P.dma("sp", xnew[i], a_[:], reads=[an], writes=[("xnew", i)])
    P.finish()
    P.emit()
    return nc


_CACHE = {}


def _prog(name, fn, *a):
    key = (name,) + a
    if key not in _CACHE:
        _CACHE[key] = fn(*a)
    return _CACHE[key]


def _run(nc, maps, tag=""):
    res = run_bass_kernel_spmd(nc, maps, core_ids=list(range(NCORES)))
    return res.results


def _rope_tables():
    t = np.arange(8192)
    row = (t // 64).astype(np.float32)
    col = (t % 64).astype(np.float32)
    inv = (np.float32(10000.0) ** (-np.arange(16, dtype=np.float32) / 16)).astype(np.float32)
    ar = row[:, None] * inv
    ac = col[:, None] * inv
    rc = np.concatenate([np.cos(ar), np.cos(ar), np.cos(ac), np.cos(ac)], 1).astype(np.float32)
    rs = np.concatenate([-np.sin(ar), np.sin(ar), -np.sin(ac), np.sin(ac)], 1).astype(np.float32)
    return rc, rs


def _ca(a):
    return np.ascontiguousarray(a)


def kernel(x, c, ctx, c_ctx, w_ada, b_ada, norm1_w, w_in, da_lambda, da_subln_w,
           ssd_conv_w, ssd_conv_b, ssd_a_log, ssd_dt_bias, ssd_d, ssd_norm_w,
           gla_gk_up, gla_gk_b, gla_norm_w, w_out, norm2_w, w_router, b_router,
           w_gate_up, b_gate_up, w_down, b_down, final_norm_w, _dbg=None):
    f32 = np.float32
    x = np.asarray(x, f32)
    ctx = np.asarray(ctx, f32)
    eye = np.eye(128, dtype=f32)
    U = np.triu(np.ones((128, 128), f32))
    Lm = _ca(U.T)
    rc, rs = _rope_tables()
    cv = np.concatenate([np.asarray(c, f32), np.asarray(c_ctx, f32)[None]], 0)
    cT = _ca(cv.reshape(3, 8, 128).transpose(2, 1, 0))
    maps = []
    for k in range(NCORES):
        ll, j = k // 4, k % 4
        maps.append({"cT": cT, "wa": _ca(w_ada[ll][:, j * ACOLS:(j + 1) * ACOLS]), "ba": _ca(b_ada[ll][None, j * ACOLS:(j + 1) * ACOLS])})
    r = _run(_prog("A", build_A), maps)
    mod = np.zeros((2, 3, 6 * D), f32)
    for k in range(NCORES):
        mod[k // 4, :, (k % 4) * ACOLS:(k % 4 + 1) * ACOLS] = r[k]["modp"]

    def mrow(l, row, chunk):
        return mod[l, row, chunk * D:(chunk + 1) * D]

    xs = []
    for k in range(NCORES):
        b, j = k // 4, k % 4
        t = np.zeros((NTI, 128, D), f32)
        t[:16] = x[b, j * 2048:(j + 1) * 2048].reshape(16, 128, D)
        t[16, :64] = ctx[b, j * 64:(j + 1) * 64]
        xs.append(t)

    for l in range(2):
        lambda_init = 0.8 - 0.6 * math.exp(-0.3 * l)
        maps = []
        for k in range(NCORES):
            b, j = k // 4, k % 4
            maps.append({"xs": xs[k], "ml": _ca(np.stack([mrow(l, b, 0), mrow(l, b, 1)])), "mc": _ca(np.stack([mrow(l, 2, 0), mrow(l, 2, 1)])),
                         "n1w": _ca(norm1_w[l][None]), "w_in": _ca(w_in[l]),
                         "rc": _ca(rc[j * 2048:(j + 1) * 2048].reshape(16, 128, 64)), "rs": _ca(rs[j * 2048:(j + 1) * 2048].reshape(16, 128, 64)), "idn": eye})
        r = _run(_prog("P", build_P), maps)
        pa = np.zeros((2, TT, INW), f32)
        for k in range(NCORES):
            b, j = k // 4, k % 4
            pr = r[k]["proj"]
            pa[b, LC + j * 2048:LC + (j + 1) * 2048] = pr[:16].reshape(2048, INW)
            pa[b, j * 64:(j + 1) * 64] = pr[16, :64]
        maps = []
        for k in range(NCORES):
            b, h = k // 4, k % 4
            maps.append({"QT": _ca(pa[b, :, h * 128:(h + 1) * 128].T), "KT": _ca(pa[b, :, 512 + h * 128:512 + (h + 1) * 128].T),
                         "V": _ca(pa[b, :, 1024 + h * 128:1024 + (h + 1) * 128].reshape(NKT, 128, 128)),
                         "lp": _ca(da_lambda[l].reshape(1, 256)), "sw": _ca(da_subln_w[l][None]), "idn": eye})
        rA = _run(_prog("MA", build_MA, lambda_init), maps)
        maps = []
        for k in range(NCORES):
            b, h = k // 4, k % 4
            g = h // 2
            xc = slice(1792 + g * 128, 1792 + (g + 1) * 128)
            Bc = slice(2048 + g * 64, 2048 + (g + 1) * 64)
            Cc = slice(2176 + g * 64, 2176 + (g + 1) * 64)

            def ch(sl):
                s2 = slice(sl.start - 1792, sl.stop - 1792)
                return _ca(ssd_conv_w[l][:, s2].T), _ca(ssd_conv_b[l][s2][:, None])
            cwx, cbx = ch(xc)
            cwB, cbB = ch(Bc)
            cwC, cbC = ch(Cc)
            sel = [(d, 2 * g + jj) for d in range(2) for jj in range(2)]
            dtcols = [2304 + d * 4 + hh for d, hh in sel]
            maps.append({"xTp": _ca(pa[b][:, xc].T), "BTp": _ca(pa[b][:, Bc].T), "CTp": _ca(pa[b][:, Cc].T),
                         "cwx": cwx, "cbx": cbx, "cwB": cwB, "cbB": cbB, "cwC": cwC, "cbC": cbC,
                         "z": _ca(pa[b][:, 1536 + g * 128:1536 + (g + 1) * 128].reshape(NKT, 128, 128)),
                         "dtr": _ca(pa[b][:, dtcols].reshape(NKT, 128, 4)),
                         "dtb": _ca(np.stack([ssd_dt_bias[l][d, hh] for d, hh in sel])[None].astype(f32)),
                         "alog": _ca(np.stack([ssd_a_log[l][d, hh] for d, hh in sel])[None].astype(f32)),
                         "dsk": _ca(np.repeat(np.asarray(ssd_d[l][2 * g:2 * g + 2], f32), 64)[None]),
                         "nw": _ca(ssd_norm_w[l][None, g * 128:(g + 1) * 128]), "U": U, "L": Lm, "idn": eye})
        rS = _run(_prog("MS", build_MS), maps)
        maps = []
        for k in range(NCORES):
            b, h = k // 4, k % 4
            q = pa[b][:, 2312 + h * 32:2312 + (h + 1) * 32]
            kk = pa[b][:, 2440 + h * 32:2440 + (h + 1) * 32]
            v = pa[b][:, 2568 + h * 64:2568 + (h + 1) * 64]
            g = pa[b][:, 2824 + h * 64:2824 + (h + 1) * 64]
            m = {"qT": _ca(q.T), "kT": _ca(kk.T), "ktok": _ca(kk.reshape(NKT, 128, 32)), "vtok": _ca(v.reshape(NKT, 128, 64)),
                 "gtok": _ca(g.reshape(NKT, 128, 64)), "nw": _ca(gla_norm_w[l][None]), "U": U, "L": Lm}
            for d in range(2):
                m[f"codeT{d}"] = _ca(pa[b][:, 3080 + d * 16:3080 + (d + 1) * 16].T)
                m[f"up{d}"] = _ca(gla_gk_up[l][d][:, h * 32:(h + 1) * 32])
                m[f"gkb{d}"] = _ca(gla_gk_b[l][d][None, h * 32:(h + 1) * 32])
            maps.append(m)
        rG = _run(_prog("MG", build_MG), maps)
        mix = np.zeros((2, TT, D), f32)
        for k in range(NCORES):
            b, h = k // 4, k % 4
            mix[b, :, h * 128:(h + 1) * 128] = rA[k]["aout"].reshape(TT, 128)
            mix[b, :, 512 + h * 64:512 + (h + 1) * 64] = rS[k]["sout"].reshape(TT, 128)[:, (h % 2) * 64:(h % 2 + 1) * 64]
            mix[b, :, 768 + h * 64:768 + (h + 1) * 64] = rG[k]["cout"].reshape(TT, 64)
        if _dbg is not None:
            _dbg[f"mix{l}"] = mix
        maps = []
        for k in range(NCORES):
            b, j = k // 4, k % 4
            mt = np.zeros((NTI, 128, D), f32)
            mt[:16] = mix[b, LC + j * 2048:LC + (j + 1) * 2048].reshape(16, 128, D)
            mt[16, :64] = mix[b, j * 64:(j + 1) * 64]
            maps.append({"xs": xs[k], "mix": mt, "mol": _ca(np.stack([mrow(l, b, q) for q in (2, 3, 4, 5)])), "moc": _ca(np.stack([mrow(l, 2, q) for q in (2, 3, 4, 5)])),
                         "n2w": _ca(norm2_w[l][None]), "w_out": _ca(w_out[l]), "wr": _ca(w_router[l]), "br": _ca(b_router[l][None]), "idn": eye})
        rO = _run(_prog("O", build_O), maps)
        if _dbg is not None:
            _dbg[f"xmid{l}"] = [rO[k]["xmid"] for k in range(NCORES)]
            _dbg[f"gates{l}"] = [rO[k]["gates"] for k in range(NCORES)]
        hT_all = np.concatenate([rO[k]["h2T"] for k in range(NCORES)], axis=2)
        gate_all = np.concatenate([rO[k]["gates"] for k in range(NCORES)], axis=0)
        maps = []
        for k in range(NCORES):
            es = slice(NEL * k, NEL * (k + 1))
            maps.append({"hT": hT_all, "gate": _ca(gate_all[:, :, es]), "wgu": _ca(w_gate_up[l][es]),
                         "bguT": _ca(np.asarray(b_gate_up[l][es], f32).reshape(NEL, 16, 128).transpose(0, 2, 1)),
                         "wd": _ca(w_down[l][es]), "bd": _ca(b_down[l][es])})
        rE = _run(_prog("E", build_E), maps)
        maps = []
        for k in range(NCORES):
            b = k // 4
            parts = np.stack([rE[s]["ffp"][k * NTI:(k + 1) * NTI] for s in range(NCORES)])
            maps.append({"xmid": rO[k]["xmid"], "parts": parts, "g2l": _ca(mrow(l, b, 5)[None]), "g2c": _ca(mrow(l, 2, 5)[None]), "fnw": _ca(final_norm_w[None])})
        rR = _run(_prog("R", build_R, l == 1), maps)
        xs = [rR[k]["xnew"] for k in range(NCORES)]
        if _dbg is not None:
            _dbg[f"xnew{l}"] = xs
    out = np.zeros((2, 8192, D), f32)
    for k in range(NCORES):
        b, j = k // 4, k % 4
        out[b, j * 2048:(j + 1) * 2048] = xs[k][:16].reshape(2048, D)
    return out
```
